# Optimizing a Trainium2 kernel written in Bass

```python
import math
import jax
import jax.numpy as jnp
from jax import lax
import numpy as np

D_MODEL = 1024
BATCH = 8
SEQ = 2048
DEPTH = 4

GRID_W = 64
CTX_LEN = 256
N_MIXERS = 3
Q_BLOCK = 128
ROPE_THETA = 10000.0
EPS = 1e-6
NEG_INF = -1e30

MLA_V = 64
MLA_HEADS = D_MODEL // MLA_V
MLA_NOPE = 64
MLA_ROPE = 32
MLA_QK = MLA_NOPE + MLA_ROPE
MLA_Q_LORA = 12 * MLA_V
MLA_KV_LORA = 4 * MLA_V

DIFF_HD = 64
DIFF_HEADS = D_MODEL // (2 * DIFF_HD)
DIFF_WIDTH = DIFF_HEADS * 2 * DIFF_HD

SWA_HD = 64
SWA_HEADS = D_MODEL // SWA_HD
SWA_KV_HEADS = 4
SWA_GROUP = SWA_HEADS // SWA_KV_HEADS
WINDOW = 128

PEER_HEADS = 8
PEER_KEYS = 128
PEER_EXPERTS = PEER_KEYS * PEER_KEYS
PEER_KEY_DIM = 128
PEER_TOPK = 16

kernel_name = 'hybrid_mla_diff_swa_peer_dit'


def rms_norm(t, g):
    tf = t.astype(jnp.float32)
    y = tf * lax.rsqrt(jnp.mean(tf * tf, axis=-1, keepdims=True) + EPS)
    return y.astype(t.dtype) * g


def modulate(t, g, shift, scale):
    return rms_norm(t, g) * (1 + scale) + shift


def rope_1d(t, pos):
    nf = t.shape[-1] // 2
    inv = ROPE_THETA ** (-jnp.arange(nf, dtype=jnp.float32) / nf)
    ang = pos.astype(jnp.float32)[:, None] * inv[None, :]
    shape = (pos.shape[0],) + (1,) * (t.ndim - 3) + (nf,)
    cos = jnp.cos(ang).reshape(shape).astype(t.dtype)
    sin = jnp.sin(ang).reshape(shape).astype(t.dtype)
    t1, t2 = t[..., :nf], t[..., nf:]
    return jnp.concatenate([t1 * cos - t2 * sin, t1 * sin + t2 * cos], axis=-1)


def axial_rope(t, row, col):
    half = t.shape[-1] // 2
    return jnp.concatenate([rope_1d(t[..., :half], row), rope_1d(t[..., half:], col)], axis=-1)


def grid_positions(n_tokens):
    rows = n_tokens // GRID_W
    row = jnp.repeat(jnp.arange(rows, dtype=jnp.int32), GRID_W)
    col = jnp.tile(jnp.arange(GRID_W, dtype=jnp.int32), rows)
    return row, col


def to_blocks(t):
    b, n = t.shape[:2]
    return jnp.moveaxis(t.reshape((b, n // Q_BLOCK, Q_BLOCK) + t.shape[2:]), 1, 0)


def from_blocks(t):
    nb, b, qb = t.shape[:3]
    return jnp.moveaxis(t, 0, 1).reshape((b, nb * qb) + t.shape[3:])


def band_blocks(t):
    b, n = t.shape[:2]
    tb = t.reshape((b, n // Q_BLOCK, Q_BLOCK) + t.shape[2:])
    tp = jnp.pad(tb, [(0, 0), (1, 1)] + [(0, 0)] * (tb.ndim - 2))
    nbhd = jnp.concatenate([tp[:, :-2], tp[:, 1:-1], tp[:, 2:]], axis=2)
    return jnp.moveaxis(nbhd, 1, 0)


def band_mask(n):
    nb = n // Q_BLOCK
    blk = jnp.arange(nb)[:, None, None]
    qpos = blk * Q_BLOCK + jnp.arange(Q_BLOCK)[None, :, None]
    kpos = (blk - 1) * Q_BLOCK + jnp.arange(3 * Q_BLOCK)[None, None, :]
    return (jnp.abs(kpos - qpos) <= WINDOW) & (kpos >= 0) & (kpos < n)


def softmax_attn(q, k, v, scale):
    s = jnp.einsum('bqhd,bkhd->bhqk', q, k).astype(jnp.float32) * scale
    p = jax.nn.softmax(s, axis=-1).astype(v.dtype)
    return jnp.einsum('bhqk,bkhd->bqhd', p, v)


def rope_tail(t, row, col):
    return jnp.concatenate([t[..., :MLA_NOPE], axial_rope(t[..., MLA_NOPE:], row, col)], axis=-1)


def mla_project(h, p, row, col, with_q):
    w_down, g_cq, g_ckv, w_uq, w_ukv, g_q, g_k = p
    b, n, _ = h.shape
    lat = h @ w_down
    cq = lat[..., :MLA_Q_LORA]
    ckv = lat[..., MLA_Q_LORA:MLA_Q_LORA + MLA_KV_LORA]
    k_rope = lat[..., MLA_Q_LORA + MLA_KV_LORA:]
    kv = (rms_norm(ckv, g_ckv) @ w_ukv).reshape(b, n, MLA_HEADS, MLA_NOPE + MLA_V)
    k = jnp.concatenate([kv[..., :MLA_NOPE],
                         jnp.broadcast_to(k_rope[:, :, None, :], (b, n, MLA_HEADS, MLA_ROPE))], axis=-1)
    k = rms_norm(k, g_k)
    v = kv[..., MLA_NOPE:]
    q = None
    if with_q:
        q = rms_norm((rms_norm(cq, g_cq) @ w_uq).reshape(b, n, MLA_HEADS, MLA_QK), g_q)
    if row is not None:
        k = rope_tail(k, row, col)
        q = rope_tail(q, row, col)
    return q, k, v


def mla_mixer(hx, hc, row, col, p, w_o, need_ctx):
    scale = MLA_QK ** -0.5
    b, n = hx.shape[:2]
    qx, kx, vx = mla_project(hx, p, row, col, True)
    qc, kc, vc = mla_project(hc, p, None, None, need_ctx)
    k_all = jnp.concatenate([kx, kc], axis=1)
    v_all = jnp.concatenate([vx, vc], axis=1)
    ox = from_blocks(lax.map(lambda qb: softmax_attn(qb, k_all, v_all, scale), to_blocks(qx)))
    yx = ox.reshape(b, n, -1) @ w_o
    yc = softmax_attn(qc, kc, vc, scale).reshape(b, hc.shape[1], -1) @ w_o if need_ctx else None
    return yx, yc


def diff_attn(q, k, v, lam, scale):
    s = jnp.einsum('bqhmd,bkhmd->bhmqk', q, k).astype(jnp.float32) * scale
    p = jax.nn.softmax(s, axis=-1)
    a = (p[:, :, 0] - lam * p[:, :, 1]).astype(v.dtype)
    return jnp.einsum('bhqk,bkhe->bqhe', a, v)


def diff_mixer(hx, hc, row, col, w_qkv, g_q, g_k, lam_p, g_sub, w_o, lam_init, need_ctx):
    scale = DIFF_HD ** -0.5
    lp = lam_p.astype(jnp.float32)
    lam = jnp.exp(jnp.sum(lp[0] * lp[1])) - jnp.exp(jnp.sum(lp[2] * lp[3])) + lam_init

    def project(h, rope, with_q):
        b, n, _ = h.shape
        qkv = h @ w_qkv
        k = rms_norm(qkv[..., DIFF_WIDTH:2 * DIFF_WIDTH].reshape(b, n, DIFF_HEADS, 2, DIFF_HD), g_k)
        v = qkv[..., 2 * DIFF_WIDTH:].reshape(b, n, DIFF_HEADS, 2 * DIFF_HD)
        q = rms_norm(qkv[..., :DIFF_WIDTH].reshape(b, n, DIFF_HEADS, 2, DIFF_HD), g_q) if with_q else None
        if rope:
            k = axial_rope(k, row, col)
            q = axial_rope(q, row, col)
        return q, k, v

    def finish(o):
        b, n = o.shape[:2]
        return (rms_norm(o, g_sub) * (1.0 - lam_init)).reshape(b, n, -1) @ w_o

    qx, kx, vx = project(hx, True, True)
    qc, kc, vc = project(hc, False, need_ctx)
    k_all = jnp.concatenate([kx, kc], axis=1)
    v_all = jnp.concatenate([vx, vc], axis=1)
    ox = from_blocks(lax.map(lambda qb: diff_attn(qb, k_all, v_all, lam, scale), to_blocks(qx)))
    yx = finish(ox)
    yc = finish(diff_attn(qc, kc, vc, lam, scale)) if need_ctx else None
    return yx, yc


def gqa_sink_attn(q, k, v, sink, scale, mask):
    s = jnp.einsum('bqhgd,bnhd->bhgqn', q, k).astype(jnp.float32) * scale
    if mask is not None:
        s = jnp.where(mask, s, NEG_INF)
    sink_col = jnp.broadcast_to(sink.astype(jnp.float32)[None, :, :, None, None], s.shape[:-1] + (1,))
    p = jax.nn.softmax(jnp.concatenate([s, sink_col], axis=-1), axis=-1)[..., :-1]
    return jnp.einsum('bhgqn,bnhd->bqhgd', p.astype(v.dtype), v)


def swa_mixer(hx, hc, row, col, w_qkv, g_q, g_k, sink, w_o, need_ctx):
    scale = SWA_HD ** -0.5
    qw = SWA_HEADS * SWA_HD
    kw = SWA_KV_HEADS * SWA_HD

    def project(h, rope, with_q):
        b, n, _ = h.shape
        qkv = h @ w_qkv
        k = rms_norm(qkv[..., qw:qw + kw].reshape(b, n, SWA_KV_HEADS, SWA_HD), g_k)
        v = qkv[..., qw + kw:].reshape(b, n, SWA_KV_HEADS, SWA_HD)
        q = rms_norm(qkv[..., :qw].reshape(b, n, SWA_KV_HEADS, SWA_GROUP, SWA_HD), g_q) if with_q else None
        if rope:
            k = axial_rope(k, row, col)
            q = axial_rope(q, row, col)
        return q, k, v

    b, n = hx.shape[:2]
    qx, kx, vx = project(hx, True, True)
    qc, kc, vc = project(hc, False, need_ctx)
    ctx_mask = jnp.ones((Q_BLOCK, hc.shape[1]), dtype=bool)

    def block(args):
        qb, kb, vb, mb = args
        return gqa_sink_attn(qb, jnp.concatenate([kb, kc], axis=1), jnp.concatenate([vb, vc], axis=1),
                             sink, scale, jnp.concatenate([mb, ctx_mask], axis=-1))

    ox = from_blocks(lax.map(block, (to_blocks(qx), band_blocks(kx), band_blocks(vx), band_mask(n))))
    yx = ox.reshape(b, n, -1) @ w_o
    yc = gqa_sink_attn(qc, kc, vc, sink, scale, None).reshape(b, hc.shape[1], -1) @ w_o if need_ctx else None
    return yx, yc


def peer(h, w_q, sub_keys, u_tab, v_tab):
    b, n, d = h.shape
    t = h.reshape(b * n, d)
    q = (t @ w_q).reshape(b * n, PEER_HEADS, 2, PEER_KEY_DIM)
    s = jnp.einsum('thcd,hcnd->thcn', q, sub_keys)
    sv, si = lax.top_k(s, PEER_TOPK)
    cand = (sv[:, :, 0, :, None] + sv[:, :, 1, None, :]).reshape(b * n, PEER_HEADS, PEER_TOPK * PEER_TOPK)
    score, pos = lax.top_k(cand, PEER_TOPK)
    i1 = jnp.take_along_axis(si[:, :, 0], pos // PEER_TOPK, axis=-1)
    i2 = jnp.take_along_axis(si[:, :, 1], pos % PEER_TOPK, axis=-1)
    expert = i1 * PEER_KEYS + i2
    gate = jax.nn.softmax(score.astype(jnp.float32), axis=-1).astype(h.dtype)

    def block(args):
        tb, eb, gb = args
        act = jax.nn.gelu(jnp.einsum('td,thkd->thk', tb, jnp.take(u_tab, eb, axis=0)), approximate=False)
        return jnp.einsum('thk,thkd->td', gb * act, jnp.take(v_tab, eb, axis=0))

    nb = (b * n) // Q_BLOCK
    out = lax.map(block, (t.reshape(nb, Q_BLOCK, d),
                          expert.reshape(nb, Q_BLOCK, PEER_HEADS, PEER_TOPK),
                          gate.reshape(nb, Q_BLOCK, PEER_HEADS, PEER_TOPK)))
    return out.reshape(b, n, d)


def setup_inputs(seed: int = 0) -> dict:
    key = jax.random.key(seed)
    ks = iter(jax.random.split(key, 48))
    n_mla = len(range(0, DEPTH, N_MIXERS))
    n_diff = len(range(1, DEPTH, N_MIXERS))
    n_swa = len(range(2, DEPTH, N_MIXERS))
    D = D_MODEL

    def nrm(shape, std=1.0):
        return jax.random.normal(next(ks), shape, jnp.float32) * std

    def w(shape, fan_in, gain=1.0):
        return nrm(shape, gain * fan_in ** -0.5)

    def g(shape):
        return 1.0 + nrm(shape, 0.02)

    return {
        'x': nrm((BATCH, SEQ, D)),
        'c': nrm((BATCH, D)),
        'ctx': nrm((BATCH, CTX_LEN, D)),
        'c_ctx': nrm((D,)),
        'ada_w': w((DEPTH, D, 6 * D), D, 0.5),
        'ada_b': nrm((DEPTH, 6 * D), 0.02),
        'norm_mix': g((DEPTH, D)),
        'norm_ffn': g((DEPTH, D)),
        'mla_w_down': w((n_mla, D, MLA_Q_LORA + MLA_KV_LORA + MLA_ROPE), D),
        'mla_g_cq': g((n_mla, MLA_Q_LORA)),
        'mla_g_ckv': g((n_mla, MLA_KV_LORA)),
        'mla_w_uq': w((n_mla, MLA_Q_LORA, MLA_HEADS * MLA_QK), MLA_Q_LORA),
        'mla_w_ukv': w((n_mla, MLA_KV_LORA, MLA_HEADS * (MLA_NOPE + MLA_V)), MLA_KV_LORA),
        'mla_g_q': g((n_mla, MLA_QK)),
        'mla_g_k': g((n_mla, MLA_QK)),
        'mla_w_o': w((n_mla, MLA_HEADS * MLA_V, D), MLA_HEADS * MLA_V),
        'diff_w_qkv': w((n_diff, D, 3 * DIFF_WIDTH), D),
        'diff_g_q': g((n_diff, DIFF_HD)),
        'diff_g_k': g((n_diff, DIFF_HD)),
        'diff_lambda': nrm((n_diff, 4, DIFF_HD), 0.1),
        'diff_g_sub': g((n_diff, 2 * DIFF_HD)),
        'diff_w_o': w((n_diff, DIFF_WIDTH, D), DIFF_WIDTH),
        'swa_w_qkv': w((n_swa, D, (SWA_HEADS + 2 * SWA_KV_HEADS) * SWA_HD), D),
        'swa_g_q': g((n_swa, SWA_HD)),
        'swa_g_k': g((n_swa, SWA_HD)),
        'swa_sink': nrm((n_swa, SWA_KV_HEADS, SWA_GROUP), 0.5),
        'swa_w_o': w((n_swa, SWA_HEADS * SWA_HD, D), SWA_HEADS * SWA_HD),
        'peer_w_q': w((DEPTH, D, PEER_HEADS * 2 * PEER_KEY_DIM), D),
        'peer_keys': w((DEPTH, PEER_HEADS, 2, PEER_KEYS, PEER_KEY_DIM), PEER_KEY_DIM),
        'peer_u': w((DEPTH, PEER_EXPERTS, D), D),
        'peer_v': w((DEPTH, PEER_EXPERTS, D), PEER_HEADS),
    }


def reference(x, c, ctx, c_ctx, ada_w, ada_b, norm_mix, norm_ffn,
              mla_w_down, mla_g_cq, mla_g_ckv, mla_w_uq, mla_w_ukv, mla_g_q, mla_g_k, mla_w_o,
              diff_w_qkv, diff_g_q, diff_g_k, diff_lambda, diff_g_sub, diff_w_o,
              swa_w_qkv, swa_g_q, swa_g_k, swa_sink, swa_w_o,
              peer_w_q, peer_keys, peer_u, peer_v):
    n_ctx = ctx.shape[1]
    row, col = grid_positions(x.shape[1])
    silu_c = jax.nn.silu(c)
    silu_cc = jax.nn.silu(c_ctx)
    for i in range(DEPTH):
        last = i == DEPTH - 1
        kind, j = i % N_MIXERS, i // N_MIXERS
        mod_x = jnp.split((silu_c @ ada_w[i] + ada_b[i])[:, None, :], 6, axis=-1)
        mod_c = jnp.split(silu_cc @ ada_w[i] + ada_b[i], 6, axis=-1)
        hx = modulate(x, norm_mix[i], mod_x[0], mod_x[1])
        hc = modulate(ctx, norm_mix[i], mod_c[0], mod_c[1])
        if kind == 0:
            p = (mla_w_down[j], mla_g_cq[j], mla_g_ckv[j], mla_w_uq[j], mla_w_ukv[j], mla_g_q[j], mla_g_k[j])
            yx, yc = mla_mixer(hx, hc, row, col, p, mla_w_o[j], not last)
        elif kind == 1:
            lam_init = 0.8 - 0.6 * math.exp(-0.3 * i)
            yx, yc = diff_mixer(hx, hc, row, col, diff_w_qkv[j], diff_g_q[j], diff_g_k[j], diff_lambda[j],
                                diff_g_sub[j], diff_w_o[j], lam_init, not last)
        else:
            yx, yc = swa_mixer(hx, hc, row, col, swa_w_qkv[j], swa_g_q[j], swa_g_k[j], swa_sink[j],
                               swa_w_o[j], not last)
        x = x + mod_x[2] * yx
        hx = modulate(x, norm_ffn[i], mod_x[3], mod_x[4])
        if last:
            x = x + mod_x[5] * peer(hx, peer_w_q[i], peer_keys[i], peer_u[i], peer_v[i])
        else:
            ctx = ctx + mod_c[2] * yc
            hc = modulate(ctx, norm_ffn[i], mod_c[3], mod_c[4])
            y = peer(jnp.concatenate([hc, hx], axis=1), peer_w_q[i], peer_keys[i], peer_u[i], peer_v[i])
            ctx = ctx + mod_c[5] * y[:, :n_ctx]
            x = x + mod_x[5] * y[:, n_ctx:]
    return x
```

```python
import math
from contextlib import ExitStack
import numpy as np
import concourse.bass as bass
import concourse.mybir as mybir
from concourse.bass_utils import run_bass_kernel_spmd

F32 = mybir.dt.float32
BF16 = mybir.dt.bfloat16
U32 = mybir.dt.uint32
AF = mybir.ActivationFunctionType
ALU = mybir.AluOpType
AX = mybir.AxisListType

D = 1024
NT = 18
NLAT = 16
TOK = NT * 128
EPS = 1e-6
NCORES = 8
DEPTH = 4


class Phase:
    def __init__(self, nc, name):
        self.nc = nc
        self.name = name
        self.ops = []
        self.lastw = {}
        self.readers = {}

    def _add(self, eng, f, r, w, kind, group=None):
        i = len(self.ops)
        deps = set()
        for k in r:
            if k in self.lastw:
                deps.add(self.lastw[k])
        for k in w:
            if k in self.lastw:
                deps.add(self.lastw[k])
            deps.update(self.readers.get(k, ()))
        self.ops.append(dict(eng=eng, f=f, kind=kind, deps=deps, group=group))
        for k in r:
            self.readers.setdefault(k, []).append(i)
        for k in w:
            self.lastw[k] = i
            self.readers[k] = []
        return i

    def op(self, eng, f, r=(), w=()):
        return self._add(eng, f, tuple(r), tuple(w), 'c')

    def dma(self, q, out, in_, r=(), w=(), group=None):
        w = tuple(w)
        r = tuple(r)
        if group is None:
            group = w[0] if w else r[0]
        return self._add(q, lambda e: e.dma_start(out=out, in_=in_), r, w, 'd', group=('g', group))

    def run(self):
        nc = self.nc
        ops = self.ops
        n = len(ops)
        need = [False] * n
        for i, o in enumerate(ops):
            for j in o['deps']:
                pj = ops[j]
                if pj['kind'] == 'c':
                    if pj['eng'] == 'pe' and o['eng'] == 'pe' and o['kind'] == 'c':
                        continue
                    need[j] = True
        last = {}
        for i, o in enumerate(ops):
            if o['kind'] == 'c':
                last[o['eng']] = i
        for e, i in last.items():
            need[i] = True
        cnt = {}
        rank = [0] * n
        for i, o in enumerate(ops):
            if o['kind'] == 'c':
                if need[i]:
                    cnt[('E', o['eng'])] = cnt.get(('E', o['eng']), 0) + 1
                    rank[i] = cnt[('E', o['eng'])]
            else:
                cnt[o['group']] = cnt.get(o['group'], 0) + 1
                rank[i] = cnt[o['group']]
        for k, v in cnt.items():
            assert v * (1 if k[0] == 'E' else 16) < 60000, (self.name, k, v)
        semmap = {}
        handles = []
        for idx, k in enumerate(cnt):
            h = nc.alloc_semaphore(name=f"{self.name}_s{idx}")
            semmap[k] = h
            handles.append(h)
        with nc.Block() as block:

            def body_for(engname):
                def body(e):
                    known = {}
                    for i, o in enumerate(ops):
                        if o['eng'] != engname:
                            continue
                        waits = {}
                        for j in o['deps']:
                            pj = ops[j]
                            if pj['kind'] == 'c':
                                if pj['eng'] == 'pe' and engname == 'pe' and o['kind'] == 'c':
                                    continue
                                s, v = ('E', pj['eng']), rank[j]
                            else:
                                s, v = pj['group'], 16 * rank[j]
                            waits[s] = max(waits.get(s, 0), v)
                        for s, v in waits.items():
                            if known.get(s, 0) >= v:
                                continue
                            e.wait_ge(semmap[s], v)
                            known[s] = v
                        ins = o['f'](e)
                        if o['kind'] == 'c':
                            if need[i]:
                                ins.then_inc(semmap[('E', engname)], 1)
                        else:
                            ins.then_inc(semmap[o['group']], 16)
                    if engname == 'sp':
                        for k, v in cnt.items():
                            e.wait_ge(semmap[k], v * (1 if k[0] == 'E' else 16))
                return body

            used = set(o['eng'] for o in ops) | {'sp'}
            reg = {'pe': block.tensor, 'act': block.scalar, 'dve': block.vector, 'pool': block.gpsimd, 'sp': block.sync}
            for en in ('sp', 'pe', 'act', 'dve', 'pool'):
                if en in used:
                    reg[en](body_for(en))
        nc.clear_and_free_semaphores(handles)
        nc.all_engine_barrier()


def bview(ap, dims):
    return bass.AP(tensor=ap.tensor, offset=ap.offset, ap=[list(ap.ap[0])] + [list(d) for d in dims])


def rope_tables(rot):
    half = rot // 2
    nf = half // 2
    inv = (10000.0 ** (-np.arange(nf, dtype=np.float32) / nf)).astype(np.float32)
    pos = np.arange(NLAT * 128)
    row = (pos // 64).astype(np.float32)
    col = (pos % 64).astype(np.float32)
    ar = row[:, None] * inv[None, :]
    ac = col[:, None] * inv[None, :]
    cr, sr = np.cos(ar).astype(np.float32), np.sin(ar).astype(np.float32)
    cc, sc = np.cos(ac).astype(np.float32), np.sin(ac).astype(np.float32)
    C = np.concatenate([cr, cr, cc, cc], axis=1)
    S = np.concatenate([-sr, sr, -sc, sc], axis=1)
    return np.stack([C, S], axis=1).reshape(NLAT, 128, 2, rot).astype(np.float32)


def swa_masks():
    m = np.zeros((6, 128, 512), np.float32)
    for d in range(6):
        kpos = (d - 1) * 128 + np.arange(128)[:, None]
        qpos = np.arange(512)[None, :]
        ok = np.abs(kpos - qpos) <= 128
        m[d] = np.where(ok, 0.0, -30000.0)
    return m


class Builder:
    def __init__(self, layers, dbg=None):
        self.layers = layers
        self.dbg = dbg
        self.nc = bass.Bass("TRN2", target_bir_lowering=False)
        self.inputs = {}
        self.T = {}

    def din(self, name, shape, dt=F32):
        self.T[name] = self.nc.dram_tensor(name, list(shape), dt, kind="ExternalInput").ap()
        return self.T[name]

    def dscr(self, name, shape, dt):
        self.T[name] = self.nc.dram_tensor(name, list(shape), dt, kind="Internal").ap()
        return self.T[name]

    def modulate_tile(self, ph, sb, t, xt, G, S, hT, tag):
        nc = self.nc
        PS = self.PS
        ss, rs, junk, tmp, hb = sb['ss'], sb['rs'], sb['junk'], sb['tmp'], sb['hb']
        ph.op('act', lambda e: e.activation(out=junk[:, 0:1024], in_=xt['ap'], func=AF.Square, accum_out=ss[:, 0:1]),
              r=[xt['k']], w=['junk', 'ss'])
        ph.op('act', lambda e: e.activation(out=rs[:, 0:1], in_=ss[:, 0:1], func=AF.Sqrt, scale=1.0 / D, bias=self.epsc[:, 0:1]),
              r=['ss'], w=['rs'])
        ph.op('dve', lambda e: e.reciprocal(out=rs[:, 0:1], in_=rs[:, 0:1]), r=['rs'], w=['rs'])
        ph.op('dve', lambda e: e.scalar_tensor_tensor(out=tmp[:, :], in0=xt['ap'], scalar=rs[:, 0:1], in1=G['ap'],
                                                      op0=ALU.mult, op1=ALU.mult), r=[xt['k'], 'rs', G['k']], w=['tmp'])
        ph.op('pool', lambda e: e.tensor_tensor(out=hb[:, :], in0=tmp[:, :], in1=S['ap'], op=ALU.add),
              r=['tmp', S['k']], w=['hb'])
        psb = PS[7][:, :].bitcast(BF16)
        for dc in range(8):
            ph.op('pe', lambda e, dc=dc: e.transpose(psb[:, dc * 128:(dc + 1) * 128], hb[:, dc * 128:(dc + 1) * 128],
                                                     self.ident[:, :]), r=['hb', 'ident'], w=['ps7'])
        ph.op('act', lambda e: e.copy(out=hT['ap'], in_=psb[:, 0:1024]), r=['ps7'], w=[hT['k']])

    def load_bcast(self, ph, dst, key, src_row):
        ph.dma('sp', dst[:, :], src_row.partition_broadcast(128), w=[key])

    def phase_mod(self, li):
        nc, T, PS = self.nc, self.T, self.PS
        with ExitStack() as es:
            ph = Phase(nc, f"md{li}")
            sbt = lambda name, shape, dt: es.enter_context(nc.sbuf_tensor(f"md{li}_{name}", shape, dt))
            cv = sbt("cv", [128, 16], F32)
            sl = sbt("sl", [128, 16], BF16)
            R = sbt("R", [2, 6144], F32)
            bias = sbt("bias", [2, 6144], F32)
            nm = sbt("nm", [2, 2048], F32)
            wb = [sbt(f"wb{k}", [128, 8, 512], BF16) for k in range(2)]
            ph.dma('sp', cv[:, :], T['cvec'], w=['cv'])
            ph.op('act', lambda e: e.activation(out=sl[:, :], in_=cv[:, :], func=AF.Silu), r=['cv'], w=['sl'])
            ph.dma('sp', bias[:, :], T['ada_b'][li, :].partition_broadcast(2), w=['bias'])
            ph.dma('sp', nm[:, 0:1024], T['norm_mix'][li, :].partition_broadcast(2), w=['nm0'])
            ph.dma('sp', nm[:, 1024:2048], T['norm_ffn'][li, :].partition_broadcast(2), w=['nm1'])
            for j in range(12):
                b = j % 2
                ph.dma('pool', wb[b][:, :, :], T['ada_w'][li, j], w=[f'wb{b}'])
                for kc in range(8):
                    ph.op('pe', lambda e, b=b, kc=kc: e.matmul(PS[b][0:2, :], sl[:, kc * 2:kc * 2 + 2], wb[b][:, kc, :],
                                                               start=(kc == 0), stop=(kc == 7)),
                          r=['sl', f'wb{b}'], w=[f'ps{b}'])
                ph.op('dve', lambda e, b=b, j=j: e.tensor_tensor(out=R[:, j * 512:(j + 1) * 512], in0=PS[b][0:2, :],
                                                                 in1=bias[:, j * 512:(j + 1) * 512], op=ALU.add),
                      r=[f'ps{b}', 'bias'], w=[f'R{j}'])
            ph.op('dve', lambda e: e.scalar_tensor_tensor(out=R[:, 1024:2048], in0=R[:, 1024:2048], scalar=1.0,
                                                          in1=nm[:, 0:1024], op0=ALU.add, op1=ALU.mult),
                  r=['R2', 'R3', 'nm0'], w=['R2', 'R3'])
            ph.op('dve', lambda e: e.scalar_tensor_tensor(out=R[:, 4096:5120], in0=R[:, 4096:5120], scalar=1.0,
                                                          in1=nm[:, 1024:2048], op0=ALU.add, op1=ALU.mult),
                  r=['R8', 'R9', 'nm1'], w=['R8', 'R9'])
            ph.dma('sp', T['modrows'][li], R[:, :], r=[f'R{j}' for j in range(12)], w=['modrows'])
            ph.run()

    def modrow(self, li, who, slot):
        return self.T['modrows'][li, who, slot * 1024:(slot + 1) * 1024]

    def attention(self, ph, es, li, kind, need_ctx):
        nc, T, PS = self.nc, self.T, self.PS
        sbt = lambda name, shape, dt: es.enter_context(nc.sbuf_tensor(f"at{li}_{name}", shape, dt))
        KTp = [sbt(f"KTp{k}", [128, TOK], BF16) for k in range(2)]
        Vp = [sbt(f"Vp{k}", [128, NT, 128], BF16) for k in range(2)]
        Qz = [[sbt(f"Qz{s}{k}", [128, 512], BF16) for k in range(2)] for s in range(2)]
        for s_ in range(2):
            for k_ in range(2):
                ph.op('dve', lambda e, s_=s_, k_=k_: e.memset(Qz[s_][k_][:, :], 0.0), w=[f'Qz{s_}{k_}'])
        PT = [sbt(f"PT{k}", [128, 512], BF16) for k in range(3)]
        OTb = [sbt(f"OTb{k}", [128, 512], BF16) for k in range(2)]
        RK = sbt("RK", [128, NT, 16], F32)
        rec = [sbt(f"rec{k}", [128, 512], F32) for k in range(2)]
        ph.dma('sp', RK[:, :, :], T['RK'].rearrange("t p h -> p t h"), r=['F1'], w=['RK'])
        if kind == 0:
            KTr = sbt("KTr", [128, TOK], BF16)
            Qr = [[sbt(f"Qr{s}{k}", [128, 512], BF16) for k in range(2)] for s in range(2)]
            ph.op('dve', lambda e: e.memset(KTr[:, :], 0.0), w=['KTr'])
            for s_ in range(2):
                for k_ in range(2):
                    ph.op('dve', lambda e, s_=s_, k_=k_: e.memset(Qr[s_][k_][:, :], 0.0), w=[f'Qr{s_}{k_}'])
            ph.dma('sp', KTr[0:32, :], T['KTr'][0:32, :], r=['F1'], w=['KTr'])
        if kind == 1:
            a0 = sbt("a0", [128, 512], F32)
            a1 = sbt("a1", [128, 512], F32)
            sqb = sbt("sqb", [128, 512], BF16)
            rstd = sbt("rstd", [128, 512], F32)
            lamt = sbt("lamt", [128, 256], F32)
            lj = sbt("lj", [128, 64], F32)
            ls = sbt("ls", [128, 4], F32)
            gsc = sbt("gsc", [128, 1], F32)
            lam_init = 0.8 - 0.6 * math.exp(-0.3 * li)
            ph.dma('sp', lamt[:, :], T['diff_lambda'].partition_broadcast(128), w=['lamt'])
            ph.dma('sp', gsc[:, :], T['diff_g_sub'].rearrange("(p o) -> p o", o=1), w=['gsc'])
            for k in range(2):
                ph.op('dve', lambda e, k=k: e.tensor_tensor(out=lj[:, :], in0=lamt[:, 2 * k * 64:(2 * k + 1) * 64], in1=lamt[:, (2 * k + 1) * 64:(2 * k + 2) * 64],
                                                            op=ALU.mult), r=['lamt'], w=['lj'])
                ph.op('dve', lambda e, k=k: e.tensor_reduce(out=ls[:, k:k + 1], in_=lj[:, :], axis=AX.X, op=ALU.add),
                      r=['lj'], w=['ls'])
            ph.op('act', lambda e: e.activation(out=ls[:, 2:4], in_=ls[:, 0:2], func=AF.Exp), r=['ls'], w=['ls'])
            ph.op('dve', lambda e: e.tensor_tensor(out=ls[:, 0:1], in0=ls[:, 3:4], in1=ls[:, 2:3], op=ALU.subtract),
                  r=['ls'], w=['ls'])
            ph.op('dve', lambda e: e.tensor_scalar(out=ls[:, 0:1], in0=ls[:, 0:1], scalar1=-lam_init, scalar2=None,
                                                   op0=ALU.add), r=['ls'], w=['ls'])
            ph.op('dve', lambda e: e.tensor_scalar(out=gsc[:, :], in0=gsc[:, :], scalar1=1.0 - lam_init, scalar2=None,
                                                   op0=ALU.mult), r=['gsc'], w=['gsc'])
        if kind == 2:
            esk = sbt("esk", [128, 16], F32)
            msk = sbt("msk", [128, 6, 512], BF16)
            ph.dma('sp', esk[:, :], T['swa_sink'].partition_broadcast(128), w=['esk'])
            ph.op('act', lambda e: e.activation(out=esk[:, :], in_=esk[:, :], func=AF.Exp), r=['esk'], w=['esk'])
            ph.dma('pool', msk[:, :, :], T['swamask'].rearrange("d k q -> k d q"), w=['msk'])

        qblocks = []
        for Q in range(4):
            if kind == 2:
                kts = [kt for kt in range(4 * Q - 1, 4 * Q + 5) if 0 <= kt < NLAT] + [16, 17]
            else:
                kts = list(range(NT))
            qblocks.append((Q * 512, 512, kts, Q))
        if need_ctx:
            qblocks.append((2048, 256, [16, 17], None))

        steps = []
        it = 0
        sidx = 0
        for p in range(8):
            for bi, (q0, qn, kts, Q) in enumerate(qblocks):
                qbuf = it % 2
                it += 1
                for s in range(2):
                    for ki, kt in enumerate(kts):
                        steps.append(dict(p=p, kb=p % 2, q0=q0, qn=qn, Q=Q, qbuf=qbuf, s=s, ki=ki, kt=kt, nk=len(kts),
                                          sbk=sidx % 2, ptk=sidx % 3, first_p=(bi == 0 and s == 0 and ki == 0),
                                          first_qb=(s == 0 and ki == 0)))
                        sidx += 1

        def emit_S(st):
            p, kb, q0, qn, Q, qbuf, s, kt, sbk = (st[k] for k in ('p', 'kb', 'q0', 'qn', 'Q', 'qbuf', 's', 'kt', 'sbk'))
            if st['first_p']:
                if kind == 2:
                    ksrc = T['KT'][p // 2]
                    vsrc = T['V'][:, (p // 2) * 128:(p // 2 + 1) * 128]
                else:
                    ksrc = T['KT'][p]
                    vsrc = T['V'][:, p * 128:(p + 1) * 128]
                ph.dma('sp', KTp[kb][:, :], ksrc, r=['F1'], w=[f'KTp{kb}'])
                ph.dma('sp', Vp[kb][:, :, :], vsrc.rearrange("(t k) c -> k t c", k=128), r=['F1'], w=[f'Vp{kb}'])
            if st['first_qb']:
                for s_ in range(2):
                    ph.dma('sp', Qz[s_][qbuf][s_ * 64:(s_ + 1) * 64, 0:qn], T['QT'][p, s_ * 64:(s_ + 1) * 64, q0:q0 + qn],
                           r=['F1'], w=[f'Qz{s_}{qbuf}'])
                    if kind == 0:
                        ph.dma('sp', Qr[s_][qbuf][0:32, 0:qn], T['QTr'][p, s_ * 32:(s_ + 1) * 32, q0:q0 + qn],
                               r=['F1'], w=[f'Qr{s_}{qbuf}'])
            extra = (kind == 0) or (kind == 2 and Q is not None and kt < NLAT)
            ph.op('pe', lambda e: e.matmul(PS[sbk][:, 0:qn], KTp[kb][:, kt * 128:(kt + 1) * 128],
                                           Qz[s][qbuf][:, 0:qn], start=True, stop=not extra),
                  r=[f'KTp{kb}', f'Qz{s}{qbuf}'], w=[f'ps{sbk}'])
            if kind == 0:
                ph.op('pe', lambda e: e.matmul(PS[sbk][:, 0:qn], KTr[:, kt * 128:(kt + 1) * 128],
                                               Qr[s][qbuf][:, 0:qn], start=False, stop=True),
                      r=['KTr', f'Qr{s}{qbuf}'], w=[f'ps{sbk}'])
            elif extra:
                d = kt - 4 * Q + 1
                ph.op('pe', lambda e: e.matmul(PS[sbk][:, 0:qn], self.ident[:, :], msk[:, d, 0:qn], start=False, stop=True),
                      r=['ident', 'msk'], w=[f'ps{sbk}'])

        def emit_rest(st):
            p, kb, q0, qn, Q, qbuf, s, kt, sbk, ptk, ki, nk = (st[k] for k in ('p', 'kb', 'q0', 'qn', 'Q', 'qbuf', 's', 'kt', 'sbk',
                                                                              'ptk', 'ki', 'nk'))
            head = 2 * p + s if kind != 2 else p // 2
            ob, db = 2 + s, 4 + s
            ph.op('act', lambda e: e.activation(out=PT[ptk][:, 0:qn], in_=PS[sbk][:, 0:qn], func=AF.Exp,
                                                scale=RK[:, kt, head:head + 1]), r=[f'ps{sbk}', 'RK'], w=[f'PT{ptk}'])
            first, lastk = ki == 0, ki == nk - 1
            ph.op('pe', lambda e: e.matmul(PS[ob][:, 0:qn], Vp[kb][:, kt, :], PT[ptk][:, 0:qn], start=first, stop=lastk),
                  r=[f'Vp{kb}', f'PT{ptk}'], w=[f'ps{ob}'])
            ph.op('pe', lambda e: e.matmul(PS[db][:, 0:qn], self.ones[:, :], PT[ptk][:, 0:qn], start=first, stop=lastk),
                  r=['ones', f'PT{ptk}'], w=[f'ps{db}'])
            if not lastk:
                return
            if kind != 1:
                rows = slice(s * 64, (s + 1) * 64)
                if kind == 0:
                    ph.op('dve', lambda e: e.reciprocal(out=rec[s][rows, 0:qn], in_=PS[db][rows, 0:qn]),
                          r=[f'ps{db}'], w=[f'rec{s}'])
                else:
                    hq = 2 * p + s
                    ph.op('dve', lambda e: e.tensor_scalar(out=rec[s][rows, 0:qn], in0=PS[db][rows, 0:qn],
                                                           scalar1=esk[rows, hq:hq + 1], scalar2=None, op0=ALU.add),
                          r=[f'ps{db}', 'esk'], w=[f'rec{s}'])
                    ph.op('dve', lambda e: e.reciprocal(out=rec[s][rows, 0:qn], in_=rec[s][rows, 0:qn]),
                          r=[f'rec{s}'], w=[f'rec{s}'])
                ph.op('dve', lambda e: e.tensor_tensor(out=OTb[qbuf][rows, 0:qn], in0=PS[ob][rows, 0:qn], in1=rec[s][rows, 0:qn],
                                                       op=ALU.mult), r=[f'ps{ob}', f'rec{s}'], w=[f'OTb{qbuf}'])
            if s == 0:
                return
            if kind == 1:
                for s2 in range(2):
                    ph.op('dve', lambda e, s2=s2: e.reciprocal(out=rec[s2][:, 0:qn], in_=PS[4 + s2][:, 0:qn]),
                          r=[f'ps{4 + s2}'], w=[f'rec{s2}'])
                ph.op('dve', lambda e: e.tensor_tensor(out=a0[:, 0:qn], in0=PS[2][:, 0:qn], in1=rec[0][:, 0:qn], op=ALU.mult),
                      r=['ps2', 'rec0'], w=['a0'])
                ph.op('dve', lambda e: e.tensor_tensor(out=a1[:, 0:qn], in0=PS[3][:, 0:qn], in1=rec[1][:, 0:qn], op=ALU.mult),
                      r=['ps3', 'rec1'], w=['a1'])
                ph.op('dve', lambda e: e.scalar_tensor_tensor(out=a0[:, 0:qn], in0=a1[:, 0:qn], scalar=ls[:, 0:1], in1=a0[:, 0:qn],
                                                              op0=ALU.mult, op1=ALU.add), r=['a0', 'a1', 'ls'], w=['a0'])
                ph.op('act', lambda e: e.activation(out=sqb[:, 0:qn], in_=a0[:, 0:qn], func=AF.Square), r=['a0'], w=['sqb'])
                ph.op('pe', lambda e: e.matmul(PS[6][:, 0:qn], self.ones[:, :], sqb[:, 0:qn], start=True, stop=True),
                      r=['ones', 'sqb'], w=['ps6'])
                ph.op('act', lambda e: e.activation(out=rstd[:, 0:qn], in_=PS[6][:, 0:qn], func=AF.Sqrt, scale=1.0 / 128,
                                                    bias=self.epsc[:, 0:1]), r=['ps6'], w=['rstd'])
                ph.op('dve', lambda e: e.reciprocal(out=rstd[:, 0:qn], in_=rstd[:, 0:qn]), r=['rstd'], w=['rstd'])
                ph.op('dve', lambda e: e.scalar_tensor_tensor(out=OTb[qbuf][:, 0:qn], in0=a0[:, 0:qn], scalar=gsc[:, 0:1],
                                                              in1=rstd[:, 0:qn], op0=ALU.mult, op1=ALU.mult),
                      r=['a0', 'gsc', 'rstd'], w=[f'OTb{qbuf}'])
            ph.dma('sp', T['OT'][p, :, q0:q0 + qn], OTb[qbuf][:, 0:qn], r=[f'OTb{qbuf}', 'F2'], w=[], group=f'OTst{qbuf}')

        emit_S(steps[0])
        for i, st in enumerate(steps):
            if i + 1 < len(steps):
                emit_S(steps[i + 1])
            emit_rest(st)

    def out_proj(self, ph, es, li, Wo, need_ctx, gx, gc, xo, yt):
        nc, T, PS = self.nc, self.T, self.PS
        sbt = lambda name, shape, dt: es.enter_context(nc.sbuf_tensor(f"op{li}_{name}", shape, dt))
        OTt = [sbt(f"OTt{k}", [128, 8, 128], BF16) for k in range(2)]
        tiles = list(range(NT if need_ctx else NLAT))
        for n, t in enumerate(tiles):
            b = n % 2
            ph.dma('sp', OTt[b][:, :, :], T['OT'][:, :, t * 128:(t + 1) * 128].rearrange("q p n -> p q n"),
                   r=['F2'], w=[f'OTt{b}'])
            ph.dma('sp', xo[b][:, :], T['X'][t * 128:(t + 1) * 128, :], r=[f'Xd{t}'], w=[f'xt{b}'])
            for half in range(2):
                for p in range(8):
                    ph.op('pe', lambda e, b=b, p=p, half=half: e.matmul(
                        PS[6 + half][:, :], OTt[b][:, p, :], Wo[:, p, half * 512:(half + 1) * 512],
                        start=(p == 0), stop=(p == 7)), r=[f'OTt{b}', 'Wo'], w=[f'ps{6 + half}'])
            g = gx if t < NLAT else gc
            for half in range(2):
                sl_ = slice(half * 512, (half + 1) * 512)
                ph.op('dve', lambda e, b=b, half=half, sl_=sl_, g=g: e.tensor_tensor(
                    out=yt[b][:, sl_], in0=PS[6 + half][:, :], in1=g['ap'][:, sl_], op=ALU.mult),
                    r=[f'ps{6 + half}', g['k']], w=['tA' if b == 0 else 'tB'])
            ph.op('pool', lambda e, b=b: e.tensor_tensor(out=xo[b][:, :], in0=xo[b][:, :], in1=yt[b][:, :], op=ALU.add),
                  r=[f'xt{b}', 'tA' if b == 0 else 'tB'], w=[f'xt{b}'])
            ph.dma('sp', T['X'][t * 128:(t + 1) * 128, :], xo[b][:, :], r=[f'xt{b}'], w=[f'Xd{t}'], group=f'Xst{b}')

    def rstd_ops(self, ph, out_ap, in_ap, n, mult_after, rk, wk):
        np_ = out_ap.shape[0]
        ph.op('act', lambda e: e.activation(out=out_ap, in_=in_ap, func=AF.Sqrt, scale=1.0 / n, bias=self.epsc[0:np_, 0:1]),
              r=rk, w=wk)
        ph.op('dve', lambda e: e.reciprocal(out=out_ap, in_=out_ap), r=wk, w=wk)
        if mult_after is not None:
            ph.op('dve', lambda e: e.tensor_scalar(out=out_ap, in0=out_ap, scalar1=float(mult_after), scalar2=None,
                                                   op0=ALU.mult), r=wk, w=wk)

    def rope_ops(self, ph, x, dst, rp, H, R, tA, tB, rk, wk):
        nf = R // 4
        Cb = rp[:, 0, :].unsqueeze(1).broadcast_to([128, H, R])
        ph.op('dve', lambda e: e.tensor_tensor(out=tA, in0=x, in1=Cb, op=ALU.mult), r=rk + ['rp'], w=['tA'])
        x5 = x.rearrange("p h (rc s j) -> p h rc s j", rc=2, s=2)
        b5 = tB.rearrange("p h (rc s j) -> p h rc s j", rc=2, s=2)
        S4 = rp[:, 1, :].rearrange("p (rc s j) -> p rc s j", rc=2, s=2)
        for s in range(2):
            Sb = S4[:, :, s, :].unsqueeze(1).broadcast_to([128, H, 2, nf])
            ph.op('pool', lambda e, s=s, Sb=Sb: e.tensor_tensor(out=b5[:, :, :, s, :], in0=x5[:, :, :, 1 - s, :], in1=Sb,
                                                                op=ALU.mult), r=rk + ['rp'], w=['tB'])
        ph.op('dve', lambda e: e.tensor_tensor(out=dst, in0=tA, in1=tB, op=ALU.add), r=['tA', 'tB'], w=wk)

    def transposes_store(self, ph, src3, nblk, width, stage, stage_key, dram_ap, psk=7, grp=None):
        psb = self.PS[psk][:, :].bitcast(BF16)
        for q in range(nblk):
            ph.op('pe', lambda e, q=q: e.transpose(psb[0:width, q * 128:(q + 1) * 128], src3[:, q, :], self.ident[:, :]),
                  r=['srcT_' + stage_key, 'ident'], w=[f'ps{psk}'])
        ph.op('act', lambda e: e.copy(out=stage[0:width, 0:nblk * 128], in_=psb[0:width, 0:nblk * 128]),
              r=[f'ps{psk}'], w=[stage_key])
        ph.dma('sp', dram_ap, stage[0:width, 0:nblk * 128].rearrange("p (q n) -> p q n", q=nblk) if nblk > 1
               else stage[0:width, 0:128], r=[stage_key, 'F1'], w=[], group=grp or ('st_' + stage_key))

    def phase_mixer(self, li):
        nc, T, PS = self.nc, self.T, self.PS
        kind, j = li % 3, li // 3
        last = li == DEPTH - 1
        need_ctx = not last
        with ExitStack() as es:
            ph = Phase(nc, f"mx{li}")
            sbt = lambda name, shape, dt: es.enter_context(nc.sbuf_tensor(f"mx{li}_{name}", shape, dt))
            bc = {}
            for nm_, who, slot in [('Gx', 0, 1), ('Sx', 0, 0), ('Gc', 1, 1), ('Sc', 1, 0), ('gx', 0, 2), ('gc', 1, 2)]:
                tl = sbt(nm_, [128, 1024], F32)
                self.load_bcast(ph, tl, nm_, self.modrow(li, who, slot))
                bc[nm_] = dict(ap=tl[:, :], k=nm_)
            sb = dict(ss=sbt("ss", [128, 4], F32), rs=sbt("rs", [128, 4], F32), junk=sbt("junk", [128, 2048], BF16),
                      tmp=sbt("tmp", [128, 1024], F32), hb=sbt("hb", [128, 1024], BF16))
            xts = [sbt(f"xt{k}", [128, 1024], F32) for k in range(2)]
            hTs = [sbt(f"hT{k}", [128, 1024], BF16) for k in range(2)]
            rp = sbt("rp", [128, 2, 64], F32)
            tA = sbt("tA", [128, 1024], F32)
            tB = sbt("tB", [128, 1024], F32)
            RKs = sbt("RKs", [128, 16], F32)
            ph.op('dve', lambda e: e.memset(RKs[:, :], 0.0), w=['RKs'])
            stQ = sbt("stQ", [128, 1024], BF16)
            stK = sbt("stK", [128, 1024], BF16)
            stR = sbt("stR", [128, 1024], BF16)
            stKr = sbt("stKr", [128, 128], BF16)
            if kind == 0:
                Wd = sbt("Wd", [128, 8, 1056], BF16)
                Wuq = sbt("Wuq", [128, 6, 1536], BF16)
                Wukv = sbt("Wukv", [128, 2, 2048], BF16)
                ph.dma('pool', Wd[:, :, :], T['mla_w_down'][j], w=['Wd'])
                ph.dma('pool', Wuq[:, :, :], T['mla_w_uq'][j], w=['Wuq'])
                ph.dma('pool', Wukv[:, :, :], T['mla_w_ukv'][j], w=['Wukv'])
                wo_src = T['mla_w_o'][j]
                gcq = sbt("gcq", [128, 768], F32)
                gckv = sbt("gckv", [128, 256], F32)
                gq = sbt("gq", [128, 96], F32)
                gk = sbt("gk", [128, 96], F32)
                self.load_bcast(ph, gcq, 'gcq', T['mla_g_cq'][j, :])
                self.load_bcast(ph, gckv, 'gckv', T['mla_g_ckv'][j, :])
                self.load_bcast(ph, gq, 'gq', T['mla_g_q'][j, :])
                self.load_bcast(ph, gk, 'gk', T['mla_g_k'][j, :])
                cqn = sbt("cqn", [128, 1024], BF16)
                cT = sbt("cT", [128, 1024], BF16)
                qf = sbt("qf", [128, 1536], F32)
                sq = sbt("sq", [128, 1536], F32)
                kvf = sbt("kvf", [128, 2048], F32)
                kro = sbt("kro", [128, 32], F32)
                st16 = sbt("st16", [128, 16], F32)
                qnp = sbt("qnp", [128, 1024], BF16)
                qtl = sbt("qtl", [128, 512], F32)
                qrb = sbt("qrb", [128, 512], BF16)
                kgb = sbt("kgb", [128, 1024], BF16)
                krd = sbt("krd", [128, 64], BF16)
                vb = sbt("vb", [128, 1024], BF16)
            else:
                ncol = 3072 if kind == 1 else 1536
                Wqkv = sbt("Wqkv", [128, 8, ncol], BF16)
                ph.dma('pool', Wqkv[:, :, :], T['diff_w_qkv' if kind == 1 else 'swa_w_qkv'], w=['Wqkv'])
                wo_src = T['diff_w_o' if kind == 1 else 'swa_w_o']
                gq = sbt("gq", [128, 64], F32)
                gk = sbt("gk", [128, 64], F32)
                self.load_bcast(ph, gq, 'gq', T['diff_g_q' if kind == 1 else 'swa_g_q'])
                self.load_bcast(ph, gk, 'gk', T['diff_g_k' if kind == 1 else 'swa_g_k'])
                qkv = sbt("qkv", [128, ncol], F32)
                sq = sbt("sq", [128, 1024], F32)
                st16 = sbt("st16", [128, 16], F32)
                qn = sbt("qn", [128, 1024], F32)
                qb = sbt("qb", [128, 1024], BF16)
                kn = sbt("kn", [128, 1024], F32)
                kb_ = sbt("kb", [128, 1024], BF16)
                vb = sbt("vb", [128, 1024], BF16)
            Wo = sbt("Wo", [128, 8, 1024], BF16)
            ph.dma('pool', Wo[:, :, :], wo_src, w=['Wo'])

            for t in range(NT):
                lat = t < NLAT
                xb = t % 2
                xt = dict(ap=xts[xb][:, :], k=f'xt{xb}')
                hT = dict(ap=hTs[xb][:, :], k=f'hT{xb}')
                ph.dma('sp', xts[xb][:, :], T['X'][t * 128:(t + 1) * 128, :], r=[f'Xd{t}'], w=[f'xt{xb}'])
                self.modulate_tile(ph, sb, t, xt, bc['Gx'] if lat else bc['Gc'], bc['Sx'] if lat else bc['Sc'], hT, 'm')
                h3 = hTs[xb][:, :].rearrange("p (c n) -> p c n", c=8)
                if kind == 0:
                    if lat:
                        ph.dma('sp', rp[:, :, 0:32], T['rope32'][t], w=['rp'])
                    for n, (c0, c1) in enumerate([(0, 512), (512, 1024), (1024, 1056)]):
                        for dc in range(8):
                            ph.op('pe', lambda e, n=n, c0=c0, c1=c1, dc=dc, h3=h3: e.matmul(
                                PS[n][:, 0:c1 - c0], h3[:, dc, :], Wd[:, dc, c0:c1], start=(dc == 0), stop=(dc == 7)),
                                r=[hT['k'], 'Wd'], w=[f'ps{n}'])
                    ss, rs, junk = sb['ss'], sb['rs'], sb['junk']
                    ph.op('act', lambda e: e.activation(out=junk[:, 0:512], in_=PS[0][:, :], func=AF.Square,
                                                        accum_out=ss[:, 1:2]), r=['ps0'], w=['junk', 'ssA'])
                    ph.op('act', lambda e: e.activation(out=junk[:, 512:768], in_=PS[1][:, 0:256], func=AF.Square,
                                                        accum_out=ss[:, 2:3]), r=['ps1'], w=['junk', 'ssB'])
                    ph.op('act', lambda e: e.activation(out=junk[:, 768:1024], in_=PS[1][:, 256:512], func=AF.Square,
                                                        accum_out=ss[:, 3:4]), r=['ps1'], w=['junk', 'ssC'])
                    ph.op('act', lambda e: e.copy(out=kro[:, :], in_=PS[2][:, 0:32]), r=['ps2'], w=['kro'])
                    ph.op('dve', lambda e: e.tensor_tensor(out=ss[:, 1:2], in0=ss[:, 1:2], in1=ss[:, 2:3], op=ALU.add),
                          r=['ssA', 'ssB'], w=['ssA'])
                    self.rstd_ops(ph, rs[:, 1:2], ss[:, 1:2], 768, None, ['ssA'], ['rsA'])
                    self.rstd_ops(ph, rs[:, 2:3], ss[:, 3:4], 256, None, ['ssC'], ['rsC'])
                    ph.op('dve', lambda e: e.scalar_tensor_tensor(out=cqn[:, 0:512], in0=PS[0][:, :], scalar=rs[:, 1:2],
                                                                  in1=gcq[:, 0:512], op0=ALU.mult, op1=ALU.mult),
                          r=['ps0', 'rsA', 'gcq'], w=['cqn'])
                    ph.op('dve', lambda e: e.scalar_tensor_tensor(out=cqn[:, 512:768], in0=PS[1][:, 0:256], scalar=rs[:, 1:2],
                                                                  in1=gcq[:, 512:768], op0=ALU.mult, op1=ALU.mult),
                          r=['ps1', 'rsA', 'gcq'], w=['cqn'])
                    ph.op('dve', lambda e: e.scalar_tensor_tensor(out=cqn[:, 768:1024], in0=PS[1][:, 256:512],
                                                                  scalar=rs[:, 2:3], in1=gckv[:, :], op0=ALU.mult,
                                                                  op1=ALU.mult), r=['ps1', 'rsC', 'gckv'], w=['cqn'])
                    psb = PS[7][:, :].bitcast(BF16)
                    for c in range(8):
                        ph.op('pe', lambda e, c=c: e.transpose(psb[:, c * 128:(c + 1) * 128], cqn[:, c * 128:(c + 1) * 128],
                                                               self.ident[:, :]), r=['cqn', 'ident'], w=['ps7'])
                    ph.op('act', lambda e: e.copy(out=cT[:, :], in_=psb[:, 0:1024]), r=['ps7'], w=['cT'])
                    c3 = cT[:, :].rearrange("p (c n) -> p c n", c=8)
                    do_q = lat or need_ctx
                    if do_q:
                        for n in range(3):
                            for c in range(6):
                                ph.op('pe', lambda e, n=n, c=c: e.matmul(PS[3 + n][:, :], c3[:, c, :],
                                                                         Wuq[:, c, n * 512:(n + 1) * 512], start=(c == 0),
                                                                         stop=(c == 5)), r=['cT', 'Wuq'], w=[f'ps{3 + n}'])
                    for n, bk in enumerate([0, 1, 2, 6]):
                        for c in range(2):
                            ph.op('pe', lambda e, n=n, bk=bk, c=c: e.matmul(PS[bk][:, :], c3[:, 6 + c, :],
                                                                            Wukv[:, c, n * 512:(n + 1) * 512], start=(c == 0),
                                                                            stop=(c == 1)), r=['cT', 'Wukv'], w=[f'ps{bk}'])
                    if do_q:
                        for n in range(3):
                            ph.op('act', lambda e, n=n: e.copy(out=qf[:, n * 512:(n + 1) * 512], in_=PS[3 + n][:, :]),
                                  r=[f'ps{3 + n}'], w=['qf'])
                        ph.op('pool', lambda e: e.tensor_tensor(out=sq[:, :], in0=qf[:, :], in1=qf[:, :], op=ALU.mult),
                              r=['qf'], w=['sq'])
                        q3 = qf[:, :].rearrange("p (h d) -> p h d", d=96)
                        ph.op('dve', lambda e: e.tensor_reduce(out=st16[:, :], in_=sq[:, :].rearrange("p (h d) -> p h d", d=96),
                                                               axis=AX.X, op=ALU.add), r=['sq'], w=['st16'])
                        self.rstd_ops(ph, st16[:, :], st16[:, :], 96, None, ['st16'], ['st16'])
                        rqb = st16[:, :].unsqueeze(2).broadcast_to([128, 16, 96])
                        ph.op('dve', lambda e: e.tensor_tensor(out=q3, in0=q3, in1=rqb, op=ALU.mult), r=['qf', 'st16'], w=['qf'])
                        gqb = gq[:, :].unsqueeze(1).broadcast_to([128, 16, 96])
                        qnp3 = qnp[:, :].rearrange("p (h d) -> p h d", d=64)
                        ph.op('pool', lambda e: e.tensor_tensor(out=qnp3, in0=q3[:, :, 0:64], in1=gqb[:, :, 0:64], op=ALU.mult),
                              r=['qf', 'gq'], w=['srcT_stQ'])
                        qtl3 = qtl[:, :].rearrange("p (h d) -> p h d", d=32)
                        qrb3 = qrb[:, :].rearrange("p (h d) -> p h d", d=32)
                        if lat:
                            ph.op('pool', lambda e: e.tensor_tensor(out=qtl3, in0=q3[:, :, 64:96], in1=gqb[:, :, 64:96],
                                                                    op=ALU.mult), r=['qf', 'gq'], w=['qtl'])
                            self.rope_ops(ph, qtl3, qrb3, rp[:, :, 0:32], 16, 32,
                                          tA[:, 0:512].rearrange("p (h d) -> p h d", d=32),
                                          tB[:, 0:512].rearrange("p (h d) -> p h d", d=32), ['qtl'], ['srcT_stR'])
                        else:
                            ph.op('pool', lambda e: e.tensor_tensor(out=qrb3, in0=q3[:, :, 64:96], in1=gqb[:, :, 64:96],
                                                                    op=ALU.mult), r=['qf', 'gq'], w=['srcT_stR'])
                        self.transposes_store(ph, qnp[:, :].rearrange("p (q c) -> p q c", q=8), 8, 128, stQ, 'stQ',
                                              T['QT'][:, :, t * 128:(t + 1) * 128].rearrange("q p n -> p q n"))
                        self.transposes_store(ph, qrb[:, :].rearrange("p (q c) -> p q c", q=8), 8, 64, stR, 'stR',
                                              T['QTr'][:, :, t * 128:(t + 1) * 128].rearrange("q p n -> p q n"))
                    for n, bk in enumerate([0, 1, 2, 6]):
                        ph.op('act', lambda e, n=n, bk=bk: e.copy(out=kvf[:, n * 512:(n + 1) * 512], in_=PS[bk][:, :]),
                              r=[f'ps{bk}'], w=['kvf'])
                    kv3 = kvf[:, :].rearrange("p (h d) -> p h d", d=128)
                    sqk3 = sq[:, 0:1024].rearrange("p (h d) -> p h d", d=64)
                    ph.op('pool', lambda e: e.tensor_tensor(out=sqk3, in0=kv3[:, :, 0:64], in1=kv3[:, :, 0:64], op=ALU.mult),
                          r=['kvf'], w=['sq'])
                    ph.op('dve', lambda e: e.tensor_reduce(out=RKs[:, :], in_=sqk3, axis=AX.X, op=ALU.add), r=['sq'], w=['RKs'])
                    ph.op('act', lambda e: e.activation(out=junk[:, 0:32], in_=kro[:, :], func=AF.Square,
                                                        accum_out=ss[:, 0:1]), r=['kro'], w=['junk', 'ss'])
                    ph.op('dve', lambda e: e.tensor_tensor(out=RKs[:, :], in0=RKs[:, :], in1=ss[:, 0:1].to_broadcast([128, 16]),
                                                           op=ALU.add), r=['RKs', 'ss'], w=['RKs'])
                    self.rstd_ops(ph, RKs[:, :], RKs[:, :], 96, 96 ** -0.5, ['RKs'], ['RKs'])
                    ph.dma('sp', T['RK'][t], RKs[:, :], r=['RKs', 'F1'], w=[], group='stRK')
                    gkb = gk[:, 0:64].unsqueeze(1).broadcast_to([128, 16, 64])
                    kgb3 = kgb[:, :].rearrange("p (h d) -> p h d", d=64)
                    ph.op('dve', lambda e: e.tensor_tensor(out=kgb3, in0=kv3[:, :, 0:64], in1=gkb, op=ALU.mult),
                          r=['kvf', 'gk'], w=['srcT_stK'])
                    ph.op('pool', lambda e: e.tensor_copy(out=vb[:, :].rearrange("p (h d) -> p h d", d=64), in_=kv3[:, :, 64:128]),
                          r=['kvf'], w=['vb'])
                    ph.dma('sp', T['V'][t * 128:(t + 1) * 128, :], vb[:, :], r=['vb', 'F1'], w=[], group='stV')
                    self.transposes_store(ph, kgb[:, :].rearrange("p (q c) -> p q c", q=8), 8, 128, stK, 'stK',
                                          T['KT'][:, :, t * 128:(t + 1) * 128].rearrange("q p n -> p q n"))
                    krg = qtl[:, 0:32]
                    ph.op('dve', lambda e: e.tensor_tensor(out=krg, in0=kro[:, :], in1=gk[:, 64:96], op=ALU.mult),
                          r=['kro', 'gk'], w=['qtl'])
                    krd3 = krd[:, :].rearrange("p (r d) -> p r d", r=2)
                    if lat:
                        self.rope_ops(ph, krg.unsqueeze(1), krd3[:, 0:1, :], rp[:, :, 0:32], 1, 32,
                                      tA[:, 0:32].unsqueeze(1), tB[:, 0:32].unsqueeze(1), ['qtl'], ['krd0', 'srcT_stKr'])
                    else:
                        ph.op('dve', lambda e: e.tensor_copy(out=krd[:, 0:32], in_=krg), r=['qtl'], w=['krd0', 'srcT_stKr'])
                    ph.op('pool', lambda e: e.tensor_copy(out=krd[:, 32:64], in_=krd[:, 0:32]), r=['krd0'], w=['srcT_stKr'])
                    self.transposes_store(ph, krd[:, :].unsqueeze(1), 1, 64, stKr, 'stKr', T['KTr'][:, t * 128:(t + 1) * 128])
                else:
                    if lat:
                        ph.dma('sp', rp[:, :, :], T['rope64'][t], w=['rp'])
                    nb = ncol // 512
                    for n in range(nb):
                        for dc in range(8):
                            ph.op('pe', lambda e, n=n, dc=dc, h3=h3: e.matmul(PS[n][:, :], h3[:, dc, :], Wqkv[:, dc, n * 512:(n + 1) * 512],
                                                                       start=(dc == 0), stop=(dc == 7)),
                                  r=[hT['k'], 'Wqkv'], w=[f'ps{n}'])
                    for n in range(nb):
                        ph.op('act', lambda e, n=n: e.copy(out=qkv[:, n * 512:(n + 1) * 512], in_=PS[n][:, :]),
                              r=[f'ps{n}'], w=['qkv'])
                    Hk = 16 if kind == 1 else 4
                    koff = 1024
                    voff = 2048 if kind == 1 else 1280
                    do_q = lat or need_ctx
                    if do_q:
                        q3 = qkv[:, 0:1024].rearrange("p (h d) -> p h d", d=64)
                        sq3 = sq[:, :].rearrange("p (h d) -> p h d", d=64)
                        ph.op('pool', lambda e: e.tensor_tensor(out=sq3, in0=q3, in1=q3, op=ALU.mult), r=['qkv'], w=['sq'])
                        ph.op('dve', lambda e: e.tensor_reduce(out=st16[:, :], in_=sq3, axis=AX.X, op=ALU.add), r=['sq'], w=['st16'])
                        self.rstd_ops(ph, st16[:, :], st16[:, :], 64, None, ['st16'], ['st16'])
                        qn3 = qn[:, :].rearrange("p (h d) -> p h d", d=64)
                        qb3 = qb[:, :].rearrange("p (h d) -> p h d", d=64)
                        ph.op('dve', lambda e: e.tensor_tensor(out=qn3, in0=q3, in1=st16[:, :].unsqueeze(2).broadcast_to([128, 16, 64]),
                                                               op=ALU.mult), r=['qkv', 'st16'], w=['qn'])
                        gqb = gq[:, :].unsqueeze(1).broadcast_to([128, 16, 64])
                        if lat:
                            ph.op('pool', lambda e: e.tensor_tensor(out=qn3, in0=qn3, in1=gqb, op=ALU.mult), r=['qn', 'gq'], w=['qn'])
                            self.rope_ops(ph, qn3, qb3, rp[:, :, :], 16, 64, tA[:, :].rearrange("p (h d) -> p h d", d=64),
                                          tB[:, :].rearrange("p (h d) -> p h d", d=64), ['qn'], ['srcT_stQ'])
                        else:
                            ph.op('pool', lambda e: e.tensor_tensor(out=qb3, in0=qn3, in1=gqb, op=ALU.mult), r=['qn', 'gq'],
                                  w=['srcT_stQ'])
                        self.transposes_store(ph, qb[:, :].rearrange("p (q c) -> p q c", q=8), 8, 128, stQ, 'stQ',
                                              T['QT'][:, :, t * 128:(t + 1) * 128].rearrange("q p n -> p q n"))
                    k3 = qkv[:, koff:koff + Hk * 64].rearrange("p (h d) -> p h d", d=64)
                    sqk3 = sq[:, 0:Hk * 64].rearrange("p (h d) -> p h d", d=64)
                    ph.op('pool', lambda e: e.tensor_tensor(out=sqk3, in0=k3, in1=k3, op=ALU.mult), r=['qkv'], w=['sq'])
                    ph.op('dve', lambda e: e.tensor_reduce(out=RKs[:, 0:Hk], in_=sqk3, axis=AX.X, op=ALU.add), r=['sq'], w=['RKs'])
                    self.rstd_ops(ph, RKs[:, 0:Hk], RKs[:, 0:Hk], 64, 64 ** -0.5, ['RKs'], ['RKs'])
                    ph.dma('sp', T['RK'][t], RKs[:, :], r=['RKs', 'F1'], w=[], group='stRK')
                    kn3 = kn[:, 0:Hk * 64].rearrange("p (h d) -> p h d", d=64)
                    gkb = gk[:, :].unsqueeze(1).broadcast_to([128, Hk, 64])
                    kkey = ['srcT_stK'] if kind == 1 else ['kb0', 'srcT_stK']
                    if kind == 1:
                        kdst = kb_[:, :].rearrange("p (h d) -> p h d", d=64)
                        kdst2 = None
                    else:
                        k4 = kb_[:, 0:512].rearrange("p (h r d) -> p h r d", r=2, d=64)
                        kdst, kdst2 = k4[:, :, 0, :], k4[:, :, 1, :]
                    if lat:
                        ph.op('pool', lambda e: e.tensor_tensor(out=kn3, in0=k3, in1=gkb, op=ALU.mult), r=['qkv', 'gk'], w=['kn'])
                        self.rope_ops(ph, kn3, kdst, rp[:, :, :], Hk, 64, tA[:, 0:Hk * 64].rearrange("p (h d) -> p h d", d=64),
                                      tB[:, 0:Hk * 64].rearrange("p (h d) -> p h d", d=64), ['kn'], kkey)
                    else:
                        ph.op('pool', lambda e: e.tensor_tensor(out=kdst, in0=k3, in1=gkb, op=ALU.mult), r=['qkv', 'gk'], w=kkey)
                    v3 = qkv[:, voff:voff + (1024 if kind == 1 else 256)]
                    if kind == 1:
                        ph.op('act', lambda e: e.copy(out=vb[:, :], in_=v3), r=['qkv'], w=['vb'])
                        ph.dma('sp', T['V'][t * 128:(t + 1) * 128, :], vb[:, :], r=['vb', 'F1'], w=[], group='stV')
                        self.transposes_store(ph, kb_[:, :].rearrange("p (q c) -> p q c", q=8), 8, 128, stK, 'stK',
                                              T['KT'][:, :, t * 128:(t + 1) * 128].rearrange("q p n -> p q n"))
                    else:
                        ph.op('pool', lambda e: e.tensor_copy(out=kdst2, in_=kdst), r=['kb0'], w=['srcT_stK'])
                        v4 = vb[:, 0:512].rearrange("p (h r d) -> p h r d", r=2, d=64)
                        vv = v3.rearrange("p (h d) -> p h d", d=64)
                        ph.op('act', lambda e: e.copy(out=v4[:, :, 0, :], in_=vv), r=['qkv'], w=['vb0', 'vb'])
                        ph.op('pool', lambda e: e.tensor_copy(out=v4[:, :, 1, :], in_=vv), r=['qkv', 'vb0'], w=['vb'])
                        ph.dma('sp', T['V'][t * 128:(t + 1) * 128, 0:512], vb[:, 0:512], r=['vb', 'F1'], w=[], group='stV')
                        self.transposes_store(ph, kb_[:, 0:512].rearrange("p (q c) -> p q c", q=4), 4, 128, stK, 'stK',
                                              T['KT'][0:4, :, t * 128:(t + 1) * 128].rearrange("q p n -> p q n"))
            fz = sbt("fz", [128, 2], F32)
            ph.op('dve', lambda e: e.memset(fz[:, 0:1], 0.0), w=['F1'])
            self.attention(ph, es, li, kind, need_ctx)
            ph.op('dve', lambda e: e.memset(fz[:, 1:2], 0.0), w=['F2'])
            self.out_proj(ph, es, li, Wo, need_ctx, bc['gx'], bc['gc'], xts, [tA, tB])
            ph.run()

    def phase_peer1(self, li):
        nc, T, PS = self.nc, self.T, self.PS
        last = li == DEPTH - 1
        tiles = list(range(NLAT if last else NT))
        with ExitStack() as es:
            ph = Phase(nc, f"pa{li}")
            sbt = lambda name, shape, dt: es.enter_context(nc.sbuf_tensor(f"pa{li}_{name}", shape, dt))
            bc = {}
            for nm_, who, slot in [('Gx', 0, 4), ('Sx', 0, 3), ('Gc', 1, 4), ('Sc', 1, 3)]:
                tl = sbt(nm_, [128, 1024], F32)
                self.load_bcast(ph, tl, nm_, self.modrow(li, who, slot))
                bc[nm_] = dict(ap=tl[:, :], k=nm_)
            sb = dict(ss=sbt("ss", [128, 4], F32), rs=sbt("rs", [128, 4], F32), junk=sbt("junk", [128, 2048], BF16),
                      tmp=sbt("tmp", [128, 1024], F32), hb=sbt("hb", [128, 1024], BF16))
            xts = [sbt(f"xt{k}", [128, 1024], F32) for k in range(2)]
            hTs = [sbt(f"hT{k}", [128, 1024], BF16) for k in range(2)]
            Wq = sbt("Wq", [128, 8, 2048], BF16)
            keysT = sbt("keysT", [128, 16, 128], BF16)
            ph.dma('pool', Wq[:, :, :], T['peer_w_q'][li], w=['Wq'])
            ph.dma('pool', keysT[:, :, :], T['peer_keys'][li], w=['keysT'])
            qTs = sbt("qTs", [128, 2048], BF16)
            sc = sbt("sc", [128, 2048], F32)
            wk2 = [sbt(f"wk{k}", [128, 256], F32) for k in range(2)]
            sv = sbt("sv", [128, 256], F32)
            si = sbt("si", [128, 256], U32)
            sif = sbt("sif", [128, 256], F32)
            cand = sbt("cand", [128, 2048], F32)
            cv = sbt("cv", [128, 128], F32)
            cp = sbt("cp", [128, 128], U32)
            cpf = sbt("cpf", [128, 128], F32)
            Af = sbt("Af", [128, 128], F32)
            Bf = sbt("Bf", [128, 128], F32)
            eq1 = sbt("eq1", [128, 2048], F32)
            eq2 = sbt("eq2", [128, 2048], F32)
            i1f = sbt("i1f", [128, 128], F32)
            i2f = sbt("i2f", [128, 128], F32)
            ge = sbt("ge", [128, 128], F32)
            gate = sbt("gate", [128, 128], F32)
            sm = sbt("sm", [128, 24], F32)
            tT = [sbt(f"tT{k}", [128, 128], F32) for k in range(3)]
            OI = [sbt(f"OI{k}", [128, 8, 128], BF16) for k in range(2)]
            OJ = [sbt(f"OJ{k}", [128, 8, 128], BF16) for k in range(2)]
            EQ = [sbt(f"EQ{k}", [128, 8, 128], BF16) for k in range(2)]
            Wt = sbt("Wt", [128, 128, 128], BF16)
            iof = self.iota
            def stage_a(n, t):
                lat = t < NLAT
                xb = n % 2
                xt = dict(ap=xts[xb][:, :], k=f'xt{xb}')
                hT = dict(ap=hTs[xb][:, :], k=f'hT{xb}')
                ph.dma('sp', xts[xb][:, :], T['X'][t * 128:(t + 1) * 128, :], w=[f'xt{xb}'])
                self.modulate_tile(ph, sb, t, xt, bc['Gx'] if lat else bc['Gc'], bc['Sx'] if lat else bc['Sc'], hT, 'f')
                h3 = hTs[xb][:, :].rearrange("p (c n) -> p c n", c=8)
                ph.dma('sp', T['H2T'][:, :, t * 128:(t + 1) * 128], h3, r=[hT['k']], w=[], group=f'stH{xb}')
                for hc in range(16):
                    bank, col = hc // 4, (hc % 4) * 128
                    for dc in range(8):
                        ph.op('pe', lambda e, bank=bank, col=col, hc=hc, dc=dc, h3=h3: e.matmul(
                            PS[bank][:, col:col + 128], Wq[:, dc, hc * 128:(hc + 1) * 128], h3[:, dc, :],
                            start=(dc == 0), stop=(dc == 7)), r=['Wq', hT['k']], w=[f'ps{bank}'])
                for bank in range(4):
                    ph.op('act', lambda e, bank=bank: e.copy(out=qTs[:, bank * 512:(bank + 1) * 512], in_=PS[bank][:, :]),
                          r=[f'ps{bank}'], w=['qTs'])
                for hc in range(16):
                    bank, col = 4 + hc // 4, (hc % 4) * 128
                    ph.op('pe', lambda e, bank=bank, col=col, hc=hc: e.matmul(
                        PS[bank][:, col:col + 128], qTs[:, hc * 128:(hc + 1) * 128], keysT[:, hc, :], start=True, stop=True),
                        r=['qTs', 'keysT'], w=[f'ps{bank}'])
                for bank in range(4):
                    ph.op('act', lambda e, bank=bank: e.copy(out=sc[:, bank * 512:(bank + 1) * 512], in_=PS[4 + bank][:, :]),
                          r=[f'ps{4 + bank}'], w=['sc'])


            def stage_b(n, t):
                def top16(src, vals, idxs, wkv, ksrc, kv, ki, kw):
                    ph.op('dve', lambda e: e.max(out=vals[:, 0:8], in_=src), r=[ksrc], w=[kv])
                    ph.op('dve', lambda e: e.max_index(out=idxs[:, 0:8], in_max=vals[:, 0:8], in_values=src),
                          r=[kv, ksrc], w=[ki])
                    ph.op('dve', lambda e: e.match_replace(out=wkv, in_to_replace=vals[:, 0:8], in_values=src,
                                                           imm_value=-1e30), r=[kv, ksrc], w=[kw])
                    ph.op('dve', lambda e: e.max(out=vals[:, 8:16], in_=wkv), r=[kw], w=[kv])
                    ph.op('dve', lambda e: e.max_index(out=idxs[:, 8:16], in_max=vals[:, 8:16], in_values=wkv),
                          r=[kv, kw], w=[ki])

                for hc in range(16):
                    top16(sc[:, hc * 128:(hc + 1) * 128], sv[:, hc * 16:(hc + 1) * 16], si[:, hc * 16:(hc + 1) * 16],
                          wk2[hc % 2][:, 0:128], 'sc', f'sv{hc}', f'si{hc}', f'wk{hc % 2}')
                ph.op('dve', lambda e: e.tensor_copy(out=sif[:, :], in_=si[:, :]), r=[f'si{k}' for k in range(16)], w=['sif'])
                sv4 = sv[:, :].rearrange("p (h c k) -> p h c k", c=2, k=16)
                ph.op('dve', lambda e: e.tensor_tensor(
                    out=cand[:, :].rearrange("p (h a b) -> p h a b", a=16, b=16),
                    in0=sv4[:, :, 0, :].unsqueeze(3).broadcast_to([128, 8, 16, 16]),
                    in1=sv4[:, :, 1, :].unsqueeze(2).broadcast_to([128, 8, 16, 16]), op=ALU.add), r=[f'sv{k}' for k in range(16)], w=['cand'])
                for h in range(8):
                    top16(cand[:, h * 256:(h + 1) * 256], cv[:, h * 16:(h + 1) * 16], cp[:, h * 16:(h + 1) * 16],
                          wk2[h % 2][:, 0:256], 'cand', f'cv{h}', f'cp{h}', f'wk{h % 2}')
                cpu = cpf[:, :].bitcast(U32)
                ph.op('dve', lambda e: e.tensor_scalar(out=cpu, in0=cp[:, :], scalar1=4, scalar2=None,
                                                       op0=ALU.logical_shift_right), r=[f'cp{k}' for k in range(8)], w=['cpf'])
                ph.op('dve', lambda e: e.tensor_copy(out=Af[:, :], in_=cpu), r=['cpf'], w=['Af'])
                ph.op('dve', lambda e: e.tensor_scalar(out=cpu, in0=cp[:, :], scalar1=15, scalar2=None,
                                                       op0=ALU.bitwise_and), r=[f'cp{k}' for k in range(8)] + ['Af'], w=['cpf'])
                ph.op('dve', lambda e: e.tensor_copy(out=Bf[:, :], in_=cpu), r=['cpf'], w=['Bf'])
                sif4 = sif[:, :].rearrange("p (h c k) -> p h c k", c=2, k=16)
                io16 = iof[:, 0:16].unsqueeze(1).unsqueeze(1).broadcast_to([128, 8, 16, 16])
                for (XF, cidx, eq, outf, key) in ((Af, 0, eq1, i1f, 'i1f'), (Bf, 1, eq2, i2f, 'i2f')):
                    eq4 = eq[:, :].rearrange("p (h a b) -> p h a b", a=16, b=16)
                    xb4 = XF[:, :].rearrange("p (h k) -> p h k", k=16).unsqueeze(3).broadcast_to([128, 8, 16, 16])
                    sb4 = sif4[:, :, cidx, :].unsqueeze(2).broadcast_to([128, 8, 16, 16])
                    ph.op('dve', lambda e, eq4=eq4, xb4=xb4: e.tensor_tensor(out=eq4, in0=io16, in1=xb4, op=ALU.is_equal),
                          r=['iota', 'Af', 'Bf'], w=[key + 'e'])
                    ph.op('pool', lambda e, eq4=eq4, sb4=sb4: e.tensor_tensor(out=eq4, in0=eq4, in1=sb4, op=ALU.mult),
                          r=[key + 'e', 'sif'], w=[key + 'e'])
                    ph.op('dve', lambda e, eq=eq, outf=outf: e.tensor_reduce(
                        out=outf[:, :], in_=eq[:, :].rearrange("p (x a) -> p x a", a=16), axis=AX.X, op=ALU.add),
                        r=[key + 'e'], w=[key])
                cv3 = cv[:, :].rearrange("p (h k) -> p h k", k=16)
                ph.op('dve', lambda e: e.tensor_scalar(out=sm[:, 0:8], in0=cv3[:, :, 0], scalar1=-1.0, scalar2=None, op0=ALU.mult),
                      r=[f'cv{k}' for k in range(8)], w=['sm'])
                for h in range(8):
                    ph.op('act', lambda e, h=h: e.activation(out=ge[:, h * 16:(h + 1) * 16], in_=cv[:, h * 16:(h + 1) * 16],
                                                             func=AF.Exp, bias=sm[:, h:h + 1], scale=1.0,
                                                             accum_out=sm[:, 8 + h:9 + h]), r=[f'cv{h}', 'sm'], w=['ge', f'Z{h}'])
                ph.op('dve', lambda e: e.reciprocal(out=sm[:, 16:24], in_=sm[:, 8:16]), r=[f'Z{h}' for h in range(8)], w=['rz'])
                ph.op('dve', lambda e: e.tensor_tensor(out=gate[:, :].rearrange("p (h k) -> p h k", k=16),
                                                       in0=ge[:, :].rearrange("p (h k) -> p h k", k=16),
                                                       in1=sm[:, 16:24].unsqueeze(2).broadcast_to([128, 8, 16]), op=ALU.mult),
                      r=['ge', 'rz'], w=['gate'])
                for k, (src, key) in enumerate(((i1f, 'i1f'), (i2f, 'i2f'), (gate, 'gate'))):
                    ph.op('pe', lambda e, k=k, src=src: e.transpose(PS[4 + k][:, 0:128], src[:, :], self.identf[:, :]),
                          r=[key, 'identf'], w=[f'ps{4 + k}'])
                    ph.op('act', lambda e, k=k: e.copy(out=tT[k][:, :], in_=PS[4 + k][:, 0:128]), r=[f'ps{4 + k}'], w=[f'tT{k}'])

            def stage_c(n, t):
                Wt3 = Wt[:, :, :]
                iob = iof[:, :].unsqueeze(1).broadcast_to([128, 8, 128])
                for g8 in range(16):
                    ob = g8 % 2
                    t0_ = g8 * 8
                    bc0 = tT[0][:, t0_:t0_ + 8].unsqueeze(2).broadcast_to([128, 8, 128])
                    bc1 = tT[1][:, t0_:t0_ + 8].unsqueeze(2).broadcast_to([128, 8, 128])
                    bc2 = tT[2][:, t0_:t0_ + 8].unsqueeze(2).broadcast_to([128, 8, 128])
                    ph.op('dve', lambda e, ob=ob, bc1=bc1: e.tensor_tensor(out=OJ[ob][:, :, :], in0=iob, in1=bc1, op=ALU.is_equal),
                          r=['iota', 'tT1'], w=[f'OJ{ob}'])
                    ph.op('dve', lambda e, ob=ob, bc0=bc0: e.tensor_tensor(out=EQ[ob][:, :, :], in0=iob, in1=bc0, op=ALU.is_equal),
                          r=['iota', 'tT0'], w=[f'EQ{ob}'])
                    ph.op('pool', lambda e, ob=ob, bc2=bc2: e.tensor_tensor(out=OI[ob][:, :, :], in0=EQ[ob][:, :, :], in1=bc2,
                                                                            op=ALU.mult), r=[f'EQ{ob}', 'tT2'], w=[f'OI{ob}'])
                    for u in range(8):
                        tt = t0_ + u
                        bank = (tt // 4) % 4
                        ph.op('pe', lambda e, ob=ob, bank=bank, tt=tt, u=u: e.matmul(
                            PS[bank][:, (tt % 4) * 128:(tt % 4 + 1) * 128], OJ[ob][:, u, :], OI[ob][:, u, :], start=True, stop=True),
                            r=[f'OI{ob}', f'OJ{ob}'], w=[f'ps{bank}'])
                        if tt % 4 == 3:
                            ph.op('act', lambda e, bank=bank, tt=tt: e.copy(
                                out=Wt3[:, :, tt - 3:tt + 1], in_=PS[bank][:, :].rearrange("p (t i) -> p i t", t=4)),
                                r=[f'ps{bank}'], w=['Wt'])
                for k8 in range(8):
                    ph.dma('sp', T['Wd2'][k8 * 16:(k8 + 1) * 16, :, t * 128:(t + 1) * 128].rearrange("i j t -> j i t"),
                           Wt3[:, k8 * 16:(k8 + 1) * 16, :], r=['Wt'], w=[], group='stW')

            stage_a(0, tiles[0])
            for n, t in enumerate(tiles):
                stage_b(n, t)
                if n + 1 < len(tiles):
                    stage_a(n + 1, tiles[n + 1])
                stage_c(n, t)
            ph.run()

    def phase_peer2(self, li):
        nc, T, PS = self.nc, self.T, self.PS
        last = li == DEPTH - 1
        ntile = NLAT if last else NT
        nblk = ntile // 2
        with ExitStack() as es:
            ph = Phase(nc, f"pb{li}")
            sbt = lambda name, shape, dt: es.enter_context(nc.sbuf_tensor(f"pb{li}_{name}", shape, dt))
            Yacc = sbt("Yacc", [128, ntile, 1024], F32)
            UTb = [sbt(f"UT{k}", [128, 8, 512], BF16) for k in range(2)]
            Vb = [sbt(f"Vb{k}", [128, 4, 1024], BF16) for k in range(2)]
            Wg = [sbt(f"Wg{k}", [128, 4, ntile * 128], BF16) for k in range(2)]
            hTb = [sbt(f"hTb{k}", [128, 8, 256], BF16) for k in range(2)]
            gl = [sbt(f"gl{k}", [128, 256], F32) for k in range(4)]
            Am = [sbt(f"Am{k}", [128, 256], BF16) for k in range(4)]
            HB = [0, 1, 6, 7]
            LOOK = 2
            items = []
            it = 0
            for g in range(32):
                for blk in range(nblk):
                    hb = it % 2
                    it += 1
                    for c in range(4):
                        items.append(dict(g=g, b=g % 2, blk=blk, hb=hb, c=c, k=len(items) % 4,
                                          first_g=(blk == 0 and c == 0), first_blk=(c == 0)))

            def emit_H(itm):
                g, b, blk, hb, c, k = (itm[x] for x in ('g', 'b', 'blk', 'hb', 'c', 'k'))
                if itm['first_g']:
                    ph.dma('pool', UTb[b][:, :, :], T['peer_u'][li, g], w=[f'UT{b}'])
                    ph.dma('pool', Vb[b][:, :, :], T['peer_v'][li, g], w=[f'Vb{b}'])
                    ph.dma('sp', Wg[b][:, :, :], T['Wd2'][g * 4:(g + 1) * 4, :, 0:ntile * 128].rearrange("c j t -> j c t"),
                           w=[f'Wg{b}'])
                if itm['first_blk']:
                    ph.dma('sp', hTb[hb][:, :, :], T['H2T'][:, :, blk * 256:(blk + 1) * 256], w=[f'hTb{hb}'])
                for dc in range(8):
                    ph.op('pe', lambda e, dc=dc: e.matmul(PS[HB[k]][:, 0:256], UTb[b][:, dc, c * 128:(c + 1) * 128], hTb[hb][:, dc, :],
                                                          start=(dc == 0), stop=(dc == 7)), r=[f'UT{b}', f'hTb{hb}'], w=[f'ps{HB[k]}'])

            def emit_out(itm):
                g, b, blk, hb, c, k = (itm[x] for x in ('g', 'b', 'blk', 'hb', 'c', 'k'))
                ph.op('act', lambda e: e.activation(out=gl[k][:, :], in_=PS[HB[k]][:, 0:256], func=AF.Gelu),
                      r=[f'ps{HB[k]}'], w=[f'gl{k}'])
                ph.op('dve', lambda e: e.tensor_tensor(out=Am[k][:, :], in0=gl[k][:, :], in1=Wg[b][:, c, blk * 256:(blk + 1) * 256],
                                                       op=ALU.mult), r=[f'gl{k}', f'Wg{b}'], w=[f'Am{k}'])
                for a in range(2):
                    for half in range(2):
                        ph.op('pe', lambda e, a=a, half=half: e.matmul(
                            PS[2 + 2 * a + half][:, :], Am[k][:, a * 128:(a + 1) * 128], Vb[b][:, c, half * 512:(half + 1) * 512],
                            start=(c == 0), stop=(c == 3)), r=[f'Am{k}', f'Vb{b}'], w=[f'ps{2 + 2 * a + half}'])
                if c != 3:
                    return
                for a in range(2):
                    tile = blk * 2 + a
                    for half in range(2):
                        ya = Yacc[:, tile, half * 512:(half + 1) * 512]
                        pk = 2 + 2 * a + half
                        if g == 0:
                            ph.op('act', lambda e, ya=ya, pk=pk: e.copy(out=ya, in_=PS[pk][:, :]), r=[f'ps{pk}'],
                                  w=[f'Y{tile}_{half}'])
                        else:
                            ph.op('dve', lambda e, ya=ya, pk=pk: e.tensor_tensor(out=ya, in0=PS[pk][:, :], in1=ya, op=ALU.add),
                                  r=[f'ps{pk}', f'Y{tile}_{half}'], w=[f'Y{tile}_{half}'])

            for i in range(min(LOOK, len(items))):
                emit_H(items[i])
            for i, itm in enumerate(items):
                if i + LOOK < len(items):
                    emit_H(items[i + LOOK])
                emit_out(itm)
            gx = sbt("gx", [128, 1024], F32)
            gc = sbt("gc", [128, 1024], F32)
            self.load_bcast(ph, gx, 'gx', self.modrow(li, 0, 5))
            self.load_bcast(ph, gc, 'gc', self.modrow(li, 1, 5))
            xo = [sbt(f"xo{k}", [128, 1024], F32) for k in range(2)]
            for t in range(ntile):
                b = t % 2
                g_ = gx if t < NLAT else gc
                gk_ = 'gx' if t < NLAT else 'gc'
                ph.dma('sp', xo[b][:, :], T['X'][t * 128:(t + 1) * 128, :], w=[f'xo{b}'])
                ph.op('dve', lambda e, t=t, g_=g_: e.tensor_tensor(out=Yacc[:, t, :], in0=Yacc[:, t, :], in1=g_[:, :], op=ALU.mult),
                      r=[f'Y{t}_0', f'Y{t}_1', gk_], w=[f'Y{t}_0', f'Y{t}_1'])
                ph.op('pool', lambda e, t=t, b=b: e.tensor_tensor(out=xo[b][:, :], in0=xo[b][:, :], in1=Yacc[:, t, :], op=ALU.add),
                      r=[f'Y{t}_0', f'Y{t}_1', f'xo{b}'], w=[f'xo{b}'])
                dst = T['out'][t * 128:(t + 1) * 128, :] if last else T['X'][t * 128:(t + 1) * 128, :]
                ph.dma('sp', dst, xo[b][:, :], r=[f'xo{b}'], w=[], group=f'stX{b}')
            ph.run()

    def phase_init(self, es):
        nc, T = self.nc, self.T
        sbt = lambda name, shape, dt: es.enter_context(nc.sbuf_tensor(name, shape, dt))
        self.ident = sbt("ident", [128, 128], BF16)
        self.identf = sbt("identf", [128, 128], F32)
        self.ones = sbt("ones", [128, 128], BF16)
        self.iota = sbt("iota", [128, 128], F32)
        self.epsc = sbt("epsc", [128, 1], F32)
        self.PS = [es.enter_context(nc.psum_tensor(f"ps{b}", [128, 512], F32)) for b in range(8)]
        ph = Phase(nc, "init")
        ph.op('dve', lambda e: e.memset(self.epsc[:, :], EPS), w=['epsc'])
        ph.dma('pool', self.ident[:, :], T['identc'], w=['ident'])
        ph.dma('sp', self.identf[:, :], T['identc'], w=['identf'])
        ph.dma('pool', self.ones[:, :], T['onesc'], w=['ones'])
        ph.dma('sp', self.iota[:, :], T['iotac'], w=['iota'])
        for t in range(NT):
            ph.dma('sp', T['X'][t * 128:(t + 1) * 128, :], T['xin'][t * 128:(t + 1) * 128, :], w=[f'X{t}'], group=f'xc{t % 4}')
        ph.run()

    def build(self):
        nc = self.nc
        L = self.layers
        kinds = set(l % 3 for l in L)
        self.din('xin', [TOK, D]); self.din('cvec', [128, 16])
        self.din('ada_w', [DEPTH, 12, 128, 8, 512]); self.din('ada_b', [DEPTH, 6144])
        self.din('norm_mix', [DEPTH, D]); self.din('norm_ffn', [DEPTH, D])
        self.din('identc', [128, 128]); self.din('onesc', [128, 128]); self.din('iotac', [128, 128])
        if 0 in kinds:
            self.din('mla_w_down', [2, 128, 8, 1056]); self.din('mla_w_uq', [2, 128, 6, 1536])
            self.din('mla_w_ukv', [2, 128, 2, 2048]); self.din('mla_w_o', [2, 128, 8, 1024])
            self.din('mla_g_cq', [2, 768]); self.din('mla_g_ckv', [2, 256]); self.din('mla_g_q', [2, 96]); self.din('mla_g_k', [2, 96])
            self.din('rope32', [NLAT, 128, 2, 32])
        if 1 in kinds:
            self.din('diff_w_qkv', [128, 8, 3072]); self.din('diff_w_o', [128, 8, 1024])
            self.din('diff_g_q', [64]); self.din('diff_g_k', [64]); self.din('diff_lambda', [256]); self.din('diff_g_sub', [128])
        if 2 in kinds:
            self.din('swa_w_qkv', [128, 8, 1536]); self.din('swa_w_o', [128, 8, 1024])
            self.din('swa_g_q', [64]); self.din('swa_g_k', [64]); self.din('swa_sink', [16]); self.din('swamask', [6, 128, 512])
        if 1 in kinds or 2 in kinds:
            self.din('rope64', [NLAT, 128, 2, 64])
        if self.peer:
            self.din('peer_w_q', [DEPTH, 128, 8, 2048]); self.din('peer_keys', [DEPTH, 128, 16, 128])
            self.din('peer_u', [DEPTH, 32, 128, 8, 512]); self.din('peer_v', [DEPTH, 32, 128, 4, 1024])
        full_out = getattr(self, 'full_out', False)
        self.T['out'] = nc.dram_tensor('out', [TOK if full_out else NLAT * 128, D], F32, kind="ExternalOutput").ap()
        self.dscr('X', [TOK, D], F32); self.dscr('modrows', [DEPTH, 2, 6144], F32)
        self.dscr('QT', [8, 128, TOK], BF16); self.dscr('QTr', [8, 64, TOK], BF16); self.dscr('KT', [8, 128, TOK], BF16)
        self.dscr('KTr', [64, TOK], BF16); self.dscr('V', [TOK, D], BF16); self.dscr('RK', [NT, 128, 16], F32)
        self.dscr('OT', [8, 128, TOK], BF16); self.dscr('H2T', [128, 8, TOK], BF16)
        if self.peer:
            self.dscr('Wd2', [128, 128, TOK], BF16)
        with ExitStack() as es:
            self.phase_init(es)
            for li in L:
                self.phase_mod(li)
                self.phase_mixer(li)
                if self.peer:
                    self.phase_peer1(li)
                    self.phase_peer2(li)
            if self.dbg or not (self.peer and (DEPTH - 1) in L):
                ph = Phase(nc, "dump")
                for t in range(NT if full_out else NLAT):
                    ph.dma('sp', self.T['out'][t * 128:(t + 1) * 128, :], self.T['X'][t * 128:(t + 1) * 128, :], w=[f'o{t}'],
                           group=f'dm{t % 4}')
                ph.run()
        return nc


def prep_shared(inp, layers, peer):
    f = lambda a: np.ascontiguousarray(a, dtype=np.float32)
    kinds = set(l % 3 for l in layers)
    sh = {}
    sh['ada_w'] = f(inp['ada_w'].reshape(DEPTH, 8, 128, 12, 512).transpose(0, 3, 2, 1, 4))
    sh['ada_b'] = f(inp['ada_b']); sh['norm_mix'] = f(inp['norm_mix']); sh['norm_ffn'] = f(inp['norm_ffn'])
    sh['identc'] = np.eye(128, dtype=np.float32); sh['onesc'] = np.ones((128, 128), np.float32)
    sh['iotac'] = f(np.tile(np.arange(128, dtype=np.float32)[None, :], (128, 1)))
    chunk = lambda w, k: f(w.reshape(w.shape[0], k, 128, w.shape[2]).transpose(0, 2, 1, 3))
    if 0 in kinds:
        sh['mla_w_down'] = chunk(inp['mla_w_down'], 8); sh['mla_w_uq'] = chunk(inp['mla_w_uq'], 6)
        sh['mla_w_ukv'] = chunk(inp['mla_w_ukv'], 2); sh['mla_w_o'] = chunk(inp['mla_w_o'], 8)
        for k in ('mla_g_cq', 'mla_g_ckv', 'mla_g_q', 'mla_g_k'):
            sh[k] = f(inp[k])
        sh['rope32'] = rope_tables(32)
    if 1 in kinds:
        sh['diff_w_qkv'] = chunk(inp['diff_w_qkv'], 8)[0]; sh['diff_w_o'] = chunk(inp['diff_w_o'], 8)[0]
        sh['diff_g_q'] = f(inp['diff_g_q'][0]); sh['diff_g_k'] = f(inp['diff_g_k'][0])
        sh['diff_lambda'] = f(inp['diff_lambda'][0].reshape(256)); sh['diff_g_sub'] = f(inp['diff_g_sub'][0])
    if 2 in kinds:
        sh['swa_w_qkv'] = chunk(inp['swa_w_qkv'], 8)[0]; sh['swa_w_o'] = chunk(inp['swa_w_o'], 8)[0]
        sh['swa_g_q'] = f(inp['swa_g_q'][0]); sh['swa_g_k'] = f(inp['swa_g_k'][0])
        sh['swa_sink'] = f(inp['swa_sink'][0].reshape(16)); sh['swamask'] = swa_masks()
    if 1 in kinds or 2 in kinds:
        sh['rope64'] = rope_tables(64)
    if peer:
        sh['peer_w_q'] = chunk(inp['peer_w_q'], 8)
        sh['peer_keys'] = f(inp['peer_keys'].transpose(0, 4, 1, 2, 3).reshape(DEPTH, 128, 16, 128))
        sh['peer_u'] = f(inp['peer_u'].reshape(DEPTH, 32, 512, 8, 128).transpose(0, 1, 4, 3, 2))
        sh['peer_v'] = f(inp['peer_v'].reshape(DEPTH, 32, 4, 128, 1024).transpose(0, 1, 3, 2, 4))
    return sh


def prep_core(inp, b):
    f = lambda a: np.ascontiguousarray(a, dtype=np.float32)
    d = {}
    d['xin'] = f(np.concatenate([inp['x'][b], inp['ctx'][b]], axis=0))
    cv = np.stack([inp['c'][b].reshape(8, 128), inp['c_ctx'].reshape(8, 128)], axis=-1)
    d['cvec'] = f(cv.transpose(1, 0, 2).reshape(128, 16))
    return d


def run(inp, layers=(0, 1, 2, 3), peer=True, cores=NCORES, dbg=False, trace=False):
    bld = Builder(list(layers), dbg)
    bld.peer = peer
    nc = bld.build()
    sh = prep_shared(inp, layers, peer)
    in_maps = []
    for b in range(cores):
        m = dict(sh)
        m.update(prep_core(inp, b))
        in_maps.append(m)
    res = run_bass_kernel_spmd(nc, in_maps, core_ids=list(range(cores)), **({'trace': True} if trace else {}))
    return np.stack([r['out'] for r in res.results], axis=0), res


def run_unfused(inp, cores=NCORES):
    sh = prep_shared(inp, (0, 1, 2, 3), True)
    xin = [prep_core(inp, b) for b in range(cores)]
    out = None
    for li in range(DEPTH):
        bld = Builder([li], False)
        bld.peer = True
        bld.full_out = li < DEPTH - 1
        nc = bld.build()
        names = set(bld.T.keys())
        in_maps = []
        for b in range(cores):
            m = {k: v for k, v in sh.items() if k in names}
            m.update(xin[b])
            in_maps.append(m)
        res = run_bass_kernel_spmd(nc, in_maps, core_ids=list(range(cores)))
        if li < DEPTH - 1:
            for b in range(cores):
                xin[b]['xin'] = np.ascontiguousarray(res.results[b]['out'], dtype=np.float32)
        else:
            out = np.stack([r['out'] for r in res.results], axis=0)
    return out


FUSED = True


def kernel(**inputs):
    inp = {k: np.asarray(v) for k, v in inputs.items()}
    if FUSED:
        out, _ = run(inp)
    else:
        out = run_unfused(inp)
    return out.astype(np.float32)
```

```python
import math
from contextlib import ExitStack
import numpy as np
import concourse.bass as bass
import concourse.mybir as mybir
from concourse.bass_utils import run_bass_kernel_spmd

F32 = mybir.dt.float32
BF16 = mybir.dt.bfloat16
U32 = mybir.dt.uint32
AF = mybir.ActivationFunctionType
ALU = mybir.AluOpType
AX = mybir.AxisListType

D = 1024
NT = 18
NLAT = 16
TOK = NT * 128
EPS = 1e-6
NCORES = 8
DEPTH = 4


class Phase:
    def __init__(self, nc, name):
        self.nc = nc
        self.name = name
        self.ops = []
        self.lastw = {}
        self.readers = {}

    def _add(self, eng, f, r, w, kind, group=None):
        i = len(self.ops)
        deps = set()
        for k in r:
            if k in self.lastw:
                deps.add(self.lastw[k])
        for k in w:
            if k in self.lastw:
                deps.add(self.lastw[k])
            deps.update(self.readers.get(k, ()))
        self.ops.append(dict(eng=eng, f=f, kind=kind, deps=deps, group=group))
        for k in r:
            self.readers.setdefault(k, []).append(i)
        for k in w:
            self.lastw[k] = i
            self.readers[k] = []
        return i

    def op(self, eng, f, r=(), w=()):
        return self._add(eng, f, tuple(r), tuple(w), 'c')

    def dma(self, q, out, in_, r=(), w=(), group=None):
        w = tuple(w)
        r = tuple(r)
        if group is None:
            group = w[0] if w else r[0]
        return self._add(q, lambda e: e.dma_start(out=out, in_=in_), r, w, 'd', group=('g', group))

    def run(self):
        nc = self.nc
        ops = self.ops
        n = len(ops)
        need = [False] * n
        for i, o in enumerate(ops):
            for j in o['deps']:
                pj = ops[j]
                if pj['kind'] == 'c':
                    if pj['eng'] == 'pe' and o['eng'] == 'pe' and o['kind'] == 'c':
                        continue
                    need[j] = True
        last = {}
        for i, o in enumerate(ops):
            if o['kind'] == 'c':
                last[o['eng']] = i
        for e, i in last.items():
            need[i] = True
        cnt = {}
        rank = [0] * n
        for i, o in enumerate(ops):
            if o['kind'] == 'c':
                if need[i]:
                    cnt[('E', o['eng'])] = cnt.get(('E', o['eng']), 0) + 1
                    rank[i] = cnt[('E', o['eng'])]
            else:
                cnt[o['group']] = cnt.get(o['group'], 0) + 1
                rank[i] = cnt[o['group']]
        for k, v in cnt.items():
            assert v * (1 if k[0] == 'E' else 16) < 60000, (self.name, k, v)
        semmap = {}
        handles = []
        for idx, k in enumerate(cnt):
            h = nc.alloc_semaphore(name=f"{self.name}_s{idx}")
            semmap[k] = h
            handles.append(h)
        with nc.Block() as block:

            def body_for(engname):
                def body(e):
                    known = {}
                    for i, o in enumerate(ops):
                        if o['eng'] != engname:
                            continue
                        waits = {}
                        for j in o['deps']:
                            pj = ops[j]
                            if pj['kind'] == 'c':
                                if pj['eng'] == 'pe' and engname == 'pe' and o['kind'] == 'c':
                                    continue
                                s, v = ('E', pj['eng']), rank[j]
                            else:
                                s, v = pj['group'], 16 * rank[j]
                            waits[s] = max(waits.get(s, 0), v)
                        for s, v in waits.items():
                            if known.get(s, 0) >= v:
                                continue
                            e.wait_ge(semmap[s], v)
                            known[s] = v
                        ins = o['f'](e)
                        if o['kind'] == 'c':
                            if need[i]:
                                ins.then_inc(semmap[('E', engname)], 1)
                        else:
                            ins.then_inc(semmap[o['group']], 16)
                    if engname == 'sp':
                        for k, v in cnt.items():
                            e.wait_ge(semmap[k], v * (1 if k[0] == 'E' else 16))
                return body

            used = set(o['eng'] for o in ops) | {'sp'}
            reg = {'pe': block.tensor, 'act': block.scalar, 'dve': block.vector, 'pool': block.gpsimd, 'sp': block.sync}
            for en in ('sp', 'pe', 'act', 'dve', 'pool'):
                if en in used:
                    reg[en](body_for(en))
        nc.clear_and_free_semaphores(handles)
        nc.all_engine_barrier()


def bview(ap, dims):
    return bass.AP(tensor=ap.tensor, offset=ap.offset, ap=[list(ap.ap[0])] + [list(d) for d in dims])


def rope_tables(rot):
    half = rot // 2
    nf = half // 2
    inv = (10000.0 ** (-np.arange(nf, dtype=np.float32) / nf)).astype(np.float32)
    pos = np.arange(NLAT * 128)
    row = (pos // 64).astype(np.float32)
    col = (pos % 64).astype(np.float32)
    ar = row[:, None] * inv[None, :]
    ac = col[:, None] * inv[None, :]
    cr, sr = np.cos(ar).astype(np.float32), np.sin(ar).astype(np.float32)
    cc, sc = np.cos(ac).astype(np.float32), np.sin(ac).astype(np.float32)
    C = np.concatenate([cr, cr, cc, cc], axis=1)
    S = np.concatenate([-sr, sr, -sc, sc], axis=1)
    return np.stack([C, S], axis=1).reshape(NLAT, 128, 2, rot).astype(np.float32)


def swa_masks():
    m = np.zeros((6, 128, 512), np.float32)
    for d in range(6):
        kpos = (d - 1) * 128 + np.arange(128)[:, None]
        qpos = np.arange(512)[None, :]
        ok = np.abs(kpos - qpos) <= 128
        m[d] = np.where(ok, 0.0, -30000.0)
    return m


class Builder:
    def __init__(self, layers, dbg=None):
        self.layers = layers
        self.dbg = dbg
        self.nc = bass.Bass("TRN2", target_bir_lowering=False)
        self.inputs = {}
        self.T = {}

    def din(self, name, shape, dt=F32):
        self.T[name] = self.nc.dram_tensor(name, list(shape), dt, kind="ExternalInput").ap()
        return self.T[name]

    def dscr(self, name, shape, dt):
        self.T[name] = self.nc.dram_tensor(name, list(shape), dt, kind="Internal").ap()
        return self.T[name]

    def modulate_tile(self, ph, sb, t, xt, G, S, hT, tag):
        nc = self.nc
        PS = self.PS
        ss, rs, junk, tmp, hb = sb['ss'], sb['rs'], sb['junk'], sb['tmp'], sb['hb']
        ph.op('act', lambda e: e.activation(out=junk[:, 0:1024], in_=xt['ap'], func=AF.Square, accum_out=ss[:, 0:1]),
              r=[xt['k']], w=['junk', 'ss'])
        ph.op('act', lambda e: e.activation(out=rs[:, 0:1], in_=ss[:, 0:1], func=AF.Sqrt, scale=1.0 / D, bias=self.epsc[:, 0:1]),
              r=['ss'], w=['rs'])
        ph.op('dve', lambda e: e.reciprocal(out=rs[:, 0:1], in_=rs[:, 0:1]), r=['rs'], w=['rs'])
        ph.op('dve', lambda e: e.scalar_tensor_tensor(out=tmp[:, :], in0=xt['ap'], scalar=rs[:, 0:1], in1=G['ap'],
                                                      op0=ALU.mult, op1=ALU.mult), r=[xt['k'], 'rs', G['k']], w=['tmp'])
        ph.op('pool', lambda e: e.tensor_tensor(out=hb[:, :], in0=tmp[:, :], in1=S['ap'], op=ALU.add),
              r=['tmp', S['k']], w=['hb'])
        psb = PS[7][:, :].bitcast(BF16)
        for dc in range(8):
            ph.op('pe', lambda e, dc=dc: e.transpose(psb[:, dc * 128:(dc + 1) * 128], hb[:, dc * 128:(dc + 1) * 128],
                                                     self.ident[:, :]), r=['hb', 'ident'], w=['ps7'])
        ph.op('act', lambda e: e.copy(out=hT['ap'], in_=psb[:, 0:1024]), r=['ps7'], w=[hT['k']])

    def load_bcast(self, ph, dst, key, src_row):
        ph.dma('sp', dst[:, :], src_row.partition_broadcast(128), w=[key])

    def phase_mod(self, li):
        nc, T, PS = self.nc, self.T, self.PS
        with ExitStack() as es:
            ph = Phase(nc, f"md{li}")
            sbt = lambda name, shape, dt: es.enter_context(nc.sbuf_tensor(f"md{li}_{name}", shape, dt))
            cv = sbt("cv", [128, 16], F32)
            sl = sbt("sl", [128, 16], BF16)
            R = sbt("R", [2, 6144], F32)
            bias = sbt("bias", [2, 6144], F32)
            nm = sbt("nm", [2, 2048], F32)
            wb = [sbt(f"wb{k}", [128, 8, 512], BF16) for k in range(2)]
            ph.dma('sp', cv[:, :], T['cvec'], w=['cv'])
            ph.op('act', lambda e: e.activation(out=sl[:, :], in_=cv[:, :], func=AF.Silu), r=['cv'], w=['sl'])
            ph.dma('sp', bias[:, :], T['ada_b'][li, :].partition_broadcast(2), w=['bias'])
            ph.dma('sp', nm[:, 0:1024], T['norm_mix'][li, :].partition_broadcast(2), w=['nm0'])
            ph.dma('sp', nm[:, 1024:2048], T['norm_ffn'][li, :].partition_broadcast(2), w=['nm1'])
            for j in range(12):
                b = j % 2
                ph.dma('pool', wb[b][:, :, :], T['ada_w'][li, j], w=[f'wb{b}'])
                for kc in range(8):
                    ph.op('pe', lambda e, b=b, kc=kc: e.matmul(PS[b][0:2, :], sl[:, kc * 2:kc * 2 + 2], wb[b][:, kc, :],
                                                               start=(kc == 0), stop=(kc == 7)),
                          r=['sl', f'wb{b}'], w=[f'ps{b}'])
                ph.op('dve', lambda e, b=b, j=j: e.tensor_tensor(out=R[:, j * 512:(j + 1) * 512], in0=PS[b][0:2, :],
                                                                 in1=bias[:, j * 512:(j + 1) * 512], op=ALU.add),
                      r=[f'ps{b}', 'bias'], w=[f'R{j}'])
            ph.op('dve', lambda e: e.scalar_tensor_tensor(out=R[:, 1024:2048], in0=R[:, 1024:2048], scalar=1.0,
                                                          in1=nm[:, 0:1024], op0=ALU.add, op1=ALU.mult),
                  r=['R2', 'R3', 'nm0'], w=['R2', 'R3'])
            ph.op('dve', lambda e: e.scalar_tensor_tensor(out=R[:, 4096:5120], in0=R[:, 4096:5120], scalar=1.0,
                                                          in1=nm[:, 1024:2048], op0=ALU.add, op1=ALU.mult),
                  r=['R8', 'R9', 'nm1'], w=['R8', 'R9'])
            ph.dma('sp', T['modrows'][li], R[:, :], r=[f'R{j}' for j in range(12)], w=['modrows'])
            ph.run()

    def modrow(self, li, who, slot):
        return self.T['modrows'][li, who, slot * 1024:(slot + 1) * 1024]

    def attention(self, ph, es, li, kind, need_ctx):
        nc, T, PS = self.nc, self.T, self.PS
        sbt = lambda name, shape, dt: es.enter_context(nc.sbuf_tensor(f"at{li}_{name}", shape, dt))
        KTp = [sbt(f"KTp{k}", [128, TOK], BF16) for k in range(2)]
        Vp = [sbt(f"Vp{k}", [128, NT, 128], BF16) for k in range(2)]
        Qz = [[sbt(f"Qz{s}{k}", [128, 512], BF16) for k in range(2)] for s in range(2)]
        for s_ in range(2):
            for k_ in range(2):
                ph.op('dve', lambda e, s_=s_, k_=k_: e.memset(Qz[s_][k_][:, :], 0.0), w=[f'Qz{s_}{k_}'])
        PT = [sbt(f"PT{k}", [128, 512], BF16) for k in range(3)]
        OTb = [sbt(f"OTb{k}", [128, 512], BF16) for k in range(2)]
        RK = sbt("RK", [128, NT, 16], F32)
        rec = [sbt(f"rec{k}", [128, 512], F32) for k in range(2)]
        ph.dma('sp', RK[:, :, :], T['RK'].rearrange("t p h -> p t h"), r=['F1'], w=['RK'])
        if kind == 0:
            KTr = sbt("KTr", [128, TOK], BF16)
            Qr = [[sbt(f"Qr{s}{k}", [128, 512], BF16) for k in range(2)] for s in range(2)]
            ph.op('dve', lambda e: e.memset(KTr[:, :], 0.0), w=['KTr'])
            for s_ in range(2):
                for k_ in range(2):
                    ph.op('dve', lambda e, s_=s_, k_=k_: e.memset(Qr[s_][k_][:, :], 0.0), w=[f'Qr{s_}{k_}'])
            ph.dma('sp', KTr[0:32, :], T['KTr'][0:32, :], r=['F1'], w=['KTr'])
        if kind == 1:
            a0 = sbt("a0", [128, 512], F32)
            a1 = sbt("a1", [128, 512], F32)
            sqb = sbt("sqb", [128, 512], BF16)
            rstd = sbt("rstd", [128, 512], F32)
            lamt = sbt("lamt", [128, 256], F32)
            lj = sbt("lj", [128, 64], F32)
            ls = sbt("ls", [128, 4], F32)
            gsc = sbt("gsc", [128, 1], F32)
            lam_init = 0.8 - 0.6 * math.exp(-0.3 * li)
            ph.dma('sp', lamt[:, :], T['diff_lambda'].partition_broadcast(128), w=['lamt'])
            ph.dma('sp', gsc[:, :], T['diff_g_sub'].rearrange("(p o) -> p o", o=1), w=['gsc'])
            for k in range(2):
                ph.op('dve', lambda e, k=k: e.tensor_tensor(out=lj[:, :], in0=lamt[:, 2 * k * 64:(2 * k + 1) * 64], in1=lamt[:, (2 * k + 1) * 64:(2 * k + 2) * 64],
                                                            op=ALU.mult), r=['lamt'], w=['lj'])
                ph.op('dve', lambda e, k=k: e.tensor_reduce(out=ls[:, k:k + 1], in_=lj[:, :], axis=AX.X, op=ALU.add),
                      r=['lj'], w=['ls'])
            ph.op('act', lambda e: e.activation(out=ls[:, 2:4], in_=ls[:, 0:2], func=AF.Exp), r=['ls'], w=['ls'])
            ph.op('dve', lambda e: e.tensor_tensor(out=ls[:, 0:1], in0=ls[:, 3:4], in1=ls[:, 2:3], op=ALU.subtract),
                  r=['ls'], w=['ls'])
            ph.op('dve', lambda e: e.tensor_scalar(out=ls[:, 0:1], in0=ls[:, 0:1], scalar1=-lam_init, scalar2=None,
                                                   op0=ALU.add), r=['ls'], w=['ls'])
            ph.op('dve', lambda e: e.tensor_scalar(out=gsc[:, :], in0=gsc[:, :], scalar1=1.0 - lam_init, scalar2=None,
                                                   op0=ALU.mult), r=['gsc'], w=['gsc'])
        if kind == 2:
            esk = sbt("esk", [128, 16], F32)
            msk = sbt("msk", [128, 6, 512], BF16)
            ph.dma('sp', esk[:, :], T['swa_sink'].partition_broadcast(128), w=['esk'])
            ph.op('act', lambda e: e.activation(out=esk[:, :], in_=esk[:, :], func=AF.Exp), r=['esk'], w=['esk'])
            ph.dma('pool', msk[:, :, :], T['swamask'].rearrange("d k q -> k d q"), w=['msk'])

        qblocks = []
        for Q in range(4):
            if kind == 2:
                kts = [kt for kt in range(4 * Q - 1, 4 * Q + 5) if 0 <= kt < NLAT] + [16, 17]
            else:
                kts = list(range(NT))
            qblocks.append((Q * 512, 512, kts, Q))
        if need_ctx:
            qblocks.append((2048, 256, [16, 17], None))

        steps = []
        it = 0
        sidx = 0
        for p in range(8):
            for bi, (q0, qn, kts, Q) in enumerate(qblocks):
                qbuf = it % 2
                it += 1
                for s in range(2):
                    for ki, kt in enumerate(kts):
                        steps.append(dict(p=p, kb=p % 2, q0=q0, qn=qn, Q=Q, qbuf=qbuf, s=s, ki=ki, kt=kt, nk=len(kts),
                                          sbk=sidx % 2, ptk=sidx % 3, first_p=(bi == 0 and s == 0 and ki == 0),
                                          first_qb=(s == 0 and ki == 0)))
                        sidx += 1

        def emit_S(st):
            p, kb, q0, qn, Q, qbuf, s, kt, sbk = (st[k] for k in ('p', 'kb', 'q0', 'qn', 'Q', 'qbuf', 's', 'kt', 'sbk'))
            if st['first_p']:
                if kind == 2:
                    ksrc = T['KT'][p // 2]
                    vsrc = T['V'][:, (p // 2) * 128:(p // 2 + 1) * 128]
                else:
                    ksrc = T['KT'][p]
                    vsrc = T['V'][:, p * 128:(p + 1) * 128]
                ph.dma('sp', KTp[kb][:, :], ksrc, r=['F1'], w=[f'KTp{kb}'])
                ph.dma('sp', Vp[kb][:, :, :], vsrc.rearrange("(t k) c -> k t c", k=128), r=['F1'], w=[f'Vp{kb}'])
            if st['first_qb']:
                for s_ in range(2):
                    ph.dma('sp', Qz[s_][qbuf][s_ * 64:(s_ + 1) * 64, 0:qn], T['QT'][p, s_ * 64:(s_ + 1) * 64, q0:q0 + qn],
                           r=['F1'], w=[f'Qz{s_}{qbuf}'])
                    if kind == 0:
                        ph.dma('sp', Qr[s_][qbuf][0:32, 0:qn], T['QTr'][p, s_ * 32:(s_ + 1) * 32, q0:q0 + qn],
                               r=['F1'], w=[f'Qr{s_}{qbuf}'])
            extra = (kind == 0) or (kind == 2 and Q is not None and kt < NLAT)
            ph.op('pe', lambda e: e.matmul(PS[sbk][:, 0:qn], KTp[kb][:, kt * 128:(kt + 1) * 128],
                                           Qz[s][qbuf][:, 0:qn], start=True, stop=not extra),
                  r=[f'KTp{kb}', f'Qz{s}{qbuf}'], w=[f'ps{sbk}'])
            if kind == 0:
                ph.op('pe', lambda e: e.matmul(PS[sbk][:, 0:qn], KTr[:, kt * 128:(kt + 1) * 128],
                                               Qr[s][qbuf][:, 0:qn], start=False, stop=True),
                      r=['KTr', f'Qr{s}{qbuf}'], w=[f'ps{sbk}'])
            elif extra:
                d = kt - 4 * Q + 1
                ph.op('pe', lambda e: e.matmul(PS[sbk][:, 0:qn], self.ident[:, :], msk[:, d, 0:qn], start=False, stop=True),
                      r=['ident', 'msk'], w=[f'ps{sbk}'])

        def emit_rest(st):
            p, kb, q0, qn, Q, qbuf, s, kt, sbk, ptk, ki, nk = (st[k] for k in ('p', 'kb', 'q0', 'qn', 'Q', 'qbuf', 's', 'kt', 'sbk',
                                                                              'ptk', 'ki', 'nk'))
            head = 2 * p + s if kind != 2 else p // 2
            ob, db = 2 + s, 4 + s
            ph.op('act', lambda e: e.activation(out=PT[ptk][:, 0:qn], in_=PS[sbk][:, 0:qn], func=AF.Exp,
                                                scale=RK[:, kt, head:head + 1]), r=[f'ps{sbk}', 'RK'], w=[f'PT{ptk}'])
            first, lastk = ki == 0, ki == nk - 1
            ph.op('pe', lambda e: e.matmul(PS[ob][:, 0:qn], Vp[kb][:, kt, :], PT[ptk][:, 0:qn], start=first, stop=lastk),
                  r=[f'Vp{kb}', f'PT{ptk}'], w=[f'ps{ob}'])
            ph.op('pe', lambda e: e.matmul(PS[db][:, 0:qn], self.ones[:, :], PT[ptk][:, 0:qn], start=first, stop=lastk),
                  r=['ones', f'PT{ptk}'], w=[f'ps{db}'])
            if not lastk:
                return
            if kind != 1:
                rows = slice(s * 64, (s + 1) * 64)
                if kind == 0:
                    ph.op('dve', lambda e: e.reciprocal(out=rec[s][rows, 0:qn], in_=PS[db][rows, 0:qn]),
                          r=[f'ps{db}'], w=[f'rec{s}'])
                else:
                    hq = 2 * p + s
                    ph.op('dve', lambda e: e.tensor_scalar(out=rec[s][rows, 0:qn], in0=PS[db][rows, 0:qn],
                                                           scalar1=esk[rows, hq:hq + 1], scalar2=None, op0=ALU.add),
                          r=[f'ps{db}', 'esk'], w=[f'rec{s}'])
                    ph.op('dve', lambda e: e.reciprocal(out=rec[s][rows, 0:qn], in_=rec[s][rows, 0:qn]),
                          r=[f'rec{s}'], w=[f'rec{s}'])
                ph.op('dve', lambda e: e.tensor_tensor(out=OTb[qbuf][rows, 0:qn], in0=PS[ob][rows, 0:qn], in1=rec[s][rows, 0:qn],
                                                       op=ALU.mult), r=[f'ps{ob}', f'rec{s}'], w=[f'OTb{qbuf}'])
            if s == 0:
                return
            if kind == 1:
                for s2 in range(2):
                    ph.op('dve', lambda e, s2=s2: e.reciprocal(out=rec[s2][:, 0:qn], in_=PS[4 + s2][:, 0:qn]),
                          r=[f'ps{4 + s2}'], w=[f'rec{s2}'])
                ph.op('dve', lambda e: e.tensor_tensor(out=a0[:, 0:qn], in0=PS[2][:, 0:qn], in1=rec[0][:, 0:qn], op=ALU.mult),
                      r=['ps2', 'rec0'], w=['a0'])
                ph.op('dve', lambda e: e.tensor_tensor(out=a1[:, 0:qn], in0=PS[3][:, 0:qn], in1=rec[1][:, 0:qn], op=ALU.mult),
                      r=['ps3', 'rec1'], w=['a1'])
                ph.op('dve', lambda e: e.scalar_tensor_tensor(out=a0[:, 0:qn], in0=a1[:, 0:qn], scalar=ls[:, 0:1], in1=a0[:, 0:qn],
                                                              op0=ALU.mult, op1=ALU.add), r=['a0', 'a1', 'ls'], w=['a0'])
                ph.op('act', lambda e: e.activation(out=sqb[:, 0:qn], in_=a0[:, 0:qn], func=AF.Square), r=['a0'], w=['sqb'])
                ph.op('pe', lambda e: e.matmul(PS[6][:, 0:qn], self.ones[:, :], sqb[:, 0:qn], start=True, stop=True),
                      r=['ones', 'sqb'], w=['ps6'])
                ph.op('act', lambda e: e.activation(out=rstd[:, 0:qn], in_=PS[6][:, 0:qn], func=AF.Sqrt, scale=1.0 / 128,
                                                    bias=self.epsc[:, 0:1]), r=['ps6'], w=['rstd'])
                ph.op('dve', lambda e: e.reciprocal(out=rstd[:, 0:qn], in_=rstd[:, 0:qn]), r=['rstd'], w=['rstd'])
                ph.op('dve', lambda e: e.scalar_tensor_tensor(out=OTb[qbuf][:, 0:qn], in0=a0[:, 0:qn], scalar=gsc[:, 0:1],
                                                              in1=rstd[:, 0:qn], op0=ALU.mult, op1=ALU.mult),
                      r=['a0', 'gsc', 'rstd'], w=[f'OTb{qbuf}'])
            ph.dma('sp', T['OT'][p, :, q0:q0 + qn], OTb[qbuf][:, 0:qn], r=[f'OTb{qbuf}', 'F2'], w=[], group=f'OTst{qbuf}')

        emit_S(steps[0])
        for i, st in enumerate(steps):
            if i + 1 < len(steps):
                emit_S(steps[i + 1])
            emit_rest(st)

    def out_proj(self, ph, es, li, Wo, need_ctx, gx, gc, xo, yt):
        nc, T, PS = self.nc, self.T, self.PS
        sbt = lambda name, shape, dt: es.enter_context(nc.sbuf_tensor(f"op{li}_{name}", shape, dt))
        OTt = [sbt(f"OTt{k}", [128, 8, 128], BF16) for k in range(2)]
        tiles = list(range(NT if need_ctx else NLAT))
        for n, t in enumerate(tiles):
            b = n % 2
            ph.dma('sp', OTt[b][:, :, :], T['OT'][:, :, t * 128:(t + 1) * 128].rearrange("q p n -> p q n"),
                   r=['F2'], w=[f'OTt{b}'])
            ph.dma('sp', xo[b][:, :], T['X'][t * 128:(t + 1) * 128, :], r=[f'Xd{t}'], w=[f'xt{b}'])
            for half in range(2):
                for p in range(8):
                    ph.op('pe', lambda e, b=b, p=p, half=half: e.matmul(
                        PS[6 + half][:, :], OTt[b][:, p, :], Wo[:, p, half * 512:(half + 1) * 512],
                        start=(p == 0), stop=(p == 7)), r=[f'OTt{b}', 'Wo'], w=[f'ps{6 + half}'])
            g = gx if t < NLAT else gc
            for half in range(2):
                sl_ = slice(half * 512, (half + 1) * 512)
                ph.op('dve', lambda e, b=b, half=half, sl_=sl_, g=g: e.tensor_tensor(
                    out=yt[b][:, sl_], in0=PS[6 + half][:, :], in1=g['ap'][:, sl_], op=ALU.mult),
                    r=[f'ps{6 + half}', g['k']], w=['tA' if b == 0 else 'tB'])
            ph.op('pool', lambda e, b=b: e.tensor_tensor(out=xo[b][:, :], in0=xo[b][:, :], in1=yt[b][:, :], op=ALU.add),
                  r=[f'xt{b}', 'tA' if b == 0 else 'tB'], w=[f'xt{b}'])
            ph.dma('sp', T['X'][t * 128:(t + 1) * 128, :], xo[b][:, :], r=[f'xt{b}'], w=[f'Xd{t}'], group=f'Xst{b}')

    def rstd_ops(self, ph, out_ap, in_ap, n, mult_after, rk, wk):
        np_ = out_ap.shape[0]
        ph.op('act', lambda e: e.activation(out=out_ap, in_=in_ap, func=AF.Sqrt, scale=1.0 / n, bias=self.epsc[0:np_, 0:1]),
              r=rk, w=wk)
        ph.op('dve', lambda e: e.reciprocal(out=out_ap, in_=out_ap), r=wk, w=wk)
        if mult_after is not None:
            ph.op('dve', lambda e: e.tensor_scalar(out=out_ap, in0=out_ap, scalar1=float(mult_after), scalar2=None,
                                                   op0=ALU.mult), r=wk, w=wk)

    def rope_ops(self, ph, x, dst, rp, H, R, tA, tB, rk, wk):
        nf = R // 4
        Cb = rp[:, 0, :].unsqueeze(1).broadcast_to([128, H, R])
        ph.op('dve', lambda e: e.tensor_tensor(out=tA, in0=x, in1=Cb, op=ALU.mult), r=rk + ['rp'], w=['tA'])
        x5 = x.rearrange("p h (rc s j) -> p h rc s j", rc=2, s=2)
        b5 = tB.rearrange("p h (rc s j) -> p h rc s j", rc=2, s=2)
        S4 = rp[:, 1, :].rearrange("p (rc s j) -> p rc s j", rc=2, s=2)
        for s in range(2):
            Sb = S4[:, :, s, :].unsqueeze(1).broadcast_to([128, H, 2, nf])
            ph.op('pool', lambda e, s=s, Sb=Sb: e.tensor_tensor(out=b5[:, :, :, s, :], in0=x5[:, :, :, 1 - s, :], in1=Sb,
                                                                op=ALU.mult), r=rk + ['rp'], w=['tB'])
        ph.op('dve', lambda e: e.tensor_tensor(out=dst, in0=tA, in1=tB, op=ALU.add), r=['tA', 'tB'], w=wk)

    def transposes_store(self, ph, src3, nblk, width, stage, stage_key, dram_ap, psk=7, grp=None):
        psb = self.PS[psk][:, :].bitcast(BF16)
        for q in range(nblk):
            ph.op('pe', lambda e, q=q: e.transpose(psb[0:width, q * 128:(q + 1) * 128], src3[:, q, :], self.ident[:, :]),
                  r=['srcT_' + stage_key, 'ident'], w=[f'ps{psk}'])
        ph.op('act', lambda e: e.copy(out=stage[0:width, 0:nblk * 128], in_=psb[0:width, 0:nblk * 128]),
              r=[f'ps{psk}'], w=[stage_key])
        ph.dma('sp', dram_ap, stage[0:width, 0:nblk * 128].rearrange("p (q n) -> p q n", q=nblk) if nblk > 1
               else stage[0:width, 0:128], r=[stage_key, 'F1'], w=[], group=grp or ('st_' + stage_key))

    def phase_mixer(self, li):
        nc, T, PS = self.nc, self.T, self.PS
        kind, j = li % 3, li // 3
        last = li == DEPTH - 1
        need_ctx = not last
        with ExitStack() as es:
            ph = Phase(nc, f"mx{li}")
            sbt = lambda name, shape, dt: es.enter_context(nc.sbuf_tensor(f"mx{li}_{name}", shape, dt))
            bc = {}
            for nm_, who, slot in [('Gx', 0, 1), ('Sx', 0, 0), ('Gc', 1, 1), ('Sc', 1, 0), ('gx', 0, 2), ('gc', 1, 2)]:
                tl = sbt(nm_, [128, 1024], F32)
                self.load_bcast(ph, tl, nm_, self.modrow(li, who, slot))
                bc[nm_] = dict(ap=tl[:, :], k=nm_)
            sb = dict(ss=sbt("ss", [128, 4], F32), rs=sbt("rs", [128, 4], F32), junk=sbt("junk", [128, 2048], BF16),
                      tmp=sbt("tmp", [128, 1024], F32), hb=sbt("hb", [128, 1024], BF16))
            xts = [sbt(f"xt{k}", [128, 1024], F32) for k in range(2)]
            hTs = [sbt(f"hT{k}", [128, 1024], BF16) for k in range(2)]
            rp = sbt("rp", [128, 2, 64], F32)
            tA = sbt("tA", [128, 1024], F32)
            tB = sbt("tB", [128, 1024], F32)
            RKs = sbt("RKs", [128, 16], F32)
            ph.op('dve', lambda e: e.memset(RKs[:, :], 0.0), w=['RKs'])
            stQ = sbt("stQ", [128, 1024], BF16)
            stK = sbt("stK", [128, 1024], BF16)
            stR = sbt("stR", [128, 1024], BF16)
            stKr = sbt("stKr", [128, 128], BF16)
            if kind == 0:
                Wd = sbt("Wd", [128, 8, 1056], BF16)
                Wuq = sbt("Wuq", [128, 6, 1536], BF16)
                Wukv = sbt("Wukv", [128, 2, 2048], BF16)
                ph.dma('pool', Wd[:, :, :], T['mla_w_down'][j], w=['Wd'])
                ph.dma('pool', Wuq[:, :, :], T['mla_w_uq'][j], w=['Wuq'])
                ph.dma('pool', Wukv[:, :, :], T['mla_w_ukv'][j], w=['Wukv'])
                wo_src = T['mla_w_o'][j]
                gcq = sbt("gcq", [128, 768], F32)
                gckv = sbt("gckv", [128, 256], F32)
                gq = sbt("gq", [128, 96], F32)
                gk = sbt("gk", [128, 96], F32)
                self.load_bcast(ph, gcq, 'gcq', T['mla_g_cq'][j, :])
                self.load_bcast(ph, gckv, 'gckv', T['mla_g_ckv'][j, :])
                self.load_bcast(ph, gq, 'gq', T['mla_g_q'][j, :])
                self.load_bcast(ph, gk, 'gk', T['mla_g_k'][j, :])
                cqn = sbt("cqn", [128, 1024], BF16)
                cT = sbt("cT", [128, 1024], BF16)
                qf = sbt("qf", [128, 1536], F32)
                sq = sbt("sq", [128, 1536], F32)
                kvf = sbt("kvf", [128, 2048], F32)
                kro = sbt("kro", [128, 32], F32)
                st16 = sbt("st16", [128, 16], F32)
                qnp = sbt("qnp", [128, 1024], BF16)
                qtl = sbt("qtl", [128, 512], F32)
                qrb = sbt("qrb", [128, 512], BF16)
                kgb = sbt("kgb", [128, 1024], BF16)
                krd = sbt("krd", [128, 64], BF16)
                vb = sbt("vb", [128, 1024], BF16)
            else:
                ncol = 3072 if kind == 1 else 1536
                Wqkv = sbt("Wqkv", [128, 8, ncol], BF16)
                ph.dma('pool', Wqkv[:, :, :], T['diff_w_qkv' if kind == 1 else 'swa_w_qkv'], w=['Wqkv'])
                wo_src = T['diff_w_o' if kind == 1 else 'swa_w_o']
                gq = sbt("gq", [128, 64], F32)
                gk = sbt("gk", [128, 64], F32)
                self.load_bcast(ph, gq, 'gq', T['diff_g_q' if kind == 1 else 'swa_g_q'])
                self.load_bcast(ph, gk, 'gk', T['diff_g_k' if kind == 1 else 'swa_g_k'])
                qkv = sbt("qkv", [128, ncol], F32)
                sq = sbt("sq", [128, 1024], F32)
                st16 = sbt("st16", [128, 16], F32)
                qn = sbt("qn", [128, 1024], F32)
                qb = sbt("qb", [128, 1024], BF16)
                kn = sbt("kn", [128, 1024], F32)
                kb_ = sbt("kb", [128, 1024], BF16)
                vb = sbt("vb", [128, 1024], BF16)
            Wo = sbt("Wo", [128, 8, 1024], BF16)
            ph.dma('pool', Wo[:, :, :], wo_src, w=['Wo'])

            for t in range(NT):
                lat = t < NLAT
                xb = t % 2
                xt = dict(ap=xts[xb][:, :], k=f'xt{xb}')
                hT = dict(ap=hTs[xb][:, :], k=f'hT{xb}')
                ph.dma('sp', xts[xb][:, :], T['X'][t * 128:(t + 1) * 128, :], r=[f'Xd{t}'], w=[f'xt{xb}'])
                self.modulate_tile(ph, sb, t, xt, bc['Gx'] if lat else bc['Gc'], bc['Sx'] if lat else bc['Sc'], hT, 'm')
                h3 = hTs[xb][:, :].rearrange("p (c n) -> p c n", c=8)
                if kind == 0:
                    if lat:
                        ph.dma('sp', rp[:, :, 0:32], T['rope32'][t], w=['rp'])
                    for n, (c0, c1) in enumerate([(0, 512), (512, 1024), (1024, 1056)]):
                        for dc in range(8):
                            ph.op('pe', lambda e, n=n, c0=c0, c1=c1, dc=dc, h3=h3: e.matmul(
                                PS[n][:, 0:c1 - c0], h3[:, dc, :], Wd[:, dc, c0:c1], start=(dc == 0), stop=(dc == 7)),
                                r=[hT['k'], 'Wd'], w=[f'ps{n}'])
                    ss, rs, junk = sb['ss'], sb['rs'], sb['junk']
                    ph.op('act', lambda e: e.activation(out=junk[:, 0:512], in_=PS[0][:, :], func=AF.Square,
                                                        accum_out=ss[:, 1:2]), r=['ps0'], w=['junk', 'ssA'])
                    ph.op('act', lambda e: e.activation(out=junk[:, 512:768], in_=PS[1][:, 0:256], func=AF.Square,
                                                        accum_out=ss[:, 2:3]), r=['ps1'], w=['junk', 'ssB'])
                    ph.op('act', lambda e: e.activation(out=junk[:, 768:1024], in_=PS[1][:, 256:512], func=AF.Square,
                                                        accum_out=ss[:, 3:4]), r=['ps1'], w=['junk', 'ssC'])
                    ph.op('act', lambda e: e.copy(out=kro[:, :], in_=PS[2][:, 0:32]), r=['ps2'], w=['kro'])
                    ph.op('dve', lambda e: e.tensor_tensor(out=ss[:, 1:2], in0=ss[:, 1:2], in1=ss[:, 2:3], op=ALU.add),
                          r=['ssA', 'ssB'], w=['ssA'])
                    self.rstd_ops(ph, rs[:, 1:2], ss[:, 1:2], 768, None, ['ssA'], ['rsA'])
                    self.rstd_ops(ph, rs[:, 2:3], ss[:, 3:4], 256, None, ['ssC'], ['rsC'])
                    ph.op('dve', lambda e: e.scalar_tensor_tensor(out=cqn[:, 0:512], in0=PS[0][:, :], scalar=rs[:, 1:2],
                                                                  in1=gcq[:, 0:512], op0=ALU.mult, op1=ALU.mult),
                          r=['ps0', 'rsA', 'gcq'], w=['cqn'])
                    ph.op('dve', lambda e: e.scalar_tensor_tensor(out=cqn[:, 512:768], in0=PS[1][:, 0:256], scalar=rs[:, 1:2],
                                                                  in1=gcq[:, 512:768], op0=ALU.mult, op1=ALU.mult),
                          r=['ps1', 'rsA', 'gcq'], w=['cqn'])
                    ph.op('dve', lambda e: e.scalar_tensor_tensor(out=cqn[:, 768:1024], in0=PS[1][:, 256:512],
                                                                  scalar=rs[:, 2:3], in1=gckv[:, :], op0=ALU.mult,
                                                                  op1=ALU.mult), r=['ps1', 'rsC', 'gckv'], w=['cqn'])
                    psb = PS[7][:, :].bitcast(BF16)
                    for c in range(8):
                        ph.op('pe', lambda e, c=c: e.transpose(psb[:, c * 128:(c + 1) * 128], cqn[:, c * 128:(c + 1) * 128],
                                                               self.ident[:, :]), r=['cqn', 'ident'], w=['ps7'])
                    ph.op('act', lambda e: e.copy(out=cT[:, :], in_=psb[:, 0:1024]), r=['ps7'], w=['cT'])
                    c3 = cT[:, :].rearrange("p (c n) -> p c n", c=8)
                    do_q = lat or need_ctx
                    if do_q:
                        for n in range(3):
                            for c in range(6):
                                ph.op('pe', lambda e, n=n, c=c: e.matmul(PS[3 + n][:, :], c3[:, c, :],
                                                                         Wuq[:, c, n * 512:(n + 1) * 512], start=(c == 0),
                                                                         stop=(c == 5)), r=['cT', 'Wuq'], w=[f'ps{3 + n}'])
                    for n, bk in enumerate([0, 1, 2, 6]):
                        for c in range(2):
                            ph.op('pe', lambda e, n=n, bk=bk, c=c: e.matmul(PS[bk][:, :], c3[:, 6 + c, :],
                                                                            Wukv[:, c, n * 512:(n + 1) * 512], start=(c == 0),
                                                                            stop=(c == 1)), r=['cT', 'Wukv'], w=[f'ps{bk}'])
                    if do_q:
                        for n in range(3):
                            ph.op('act', lambda e, n=n: e.copy(out=qf[:, n * 512:(n + 1) * 512], in_=PS[3 + n][:, :]),
                                  r=[f'ps{3 + n}'], w=['qf'])
                        ph.op('pool', lambda e: e.tensor_tensor(out=sq[:, :], in0=qf[:, :], in1=qf[:, :], op=ALU.mult),
                              r=['qf'], w=['sq'])
                        q3 = qf[:, :].rearrange("p (h d) -> p h d", d=96)
                        ph.op('dve', lambda e: e.tensor_reduce(out=st16[:, :], in_=sq[:, :].rearrange("p (h d) -> p h d", d=96),
                                                               axis=AX.X, op=ALU.add), r=['sq'], w=['st16'])
                        self.rstd_ops(ph, st16[:, :], st16[:, :], 96, None, ['st16'], ['st16'])
                        rqb = st16[:, :].unsqueeze(2).broadcast_to([128, 16, 96])
                        ph.op('dve', lambda e: e.tensor_tensor(out=q3, in0=q3, in1=rqb, op=ALU.mult), r=['qf', 'st16'], w=['qf'])
                        gqb = gq[:, :].unsqueeze(1).broadcast_to([128, 16, 96])
                        qnp3 = qnp[:, :].rearrange("p (h d) -> p h d", d=64)
                        ph.op('pool', lambda e: e.tensor_tensor(out=qnp3, in0=q3[:, :, 0:64], in1=gqb[:, :, 0:64], op=ALU.mult),
                              r=['qf', 'gq'], w=['srcT_stQ'])
                        qtl3 = qtl[:, :].rearrange("p (h d) -> p h d", d=32)
                        qrb3 = qrb[:, :].rearrange("p (h d) -> p h d", d=32)
                        if lat:
                            ph.op('pool', lambda e: e.tensor_tensor(out=qtl3, in0=q3[:, :, 64:96], in1=gqb[:, :, 64:96],
                                                                    op=ALU.mult), r=['qf', 'gq'], w=['qtl'])
                            self.rope_ops(ph, qtl3, qrb3, rp[:, :, 0:32], 16, 32,
                                          tA[:, 0:512].rearrange("p (h d) -> p h d", d=32),
                                          tB[:, 0:512].rearrange("p (h d) -> p h d", d=32), ['qtl'], ['srcT_stR'])
                        else:
                            ph.op('pool', lambda e: e.tensor_tensor(out=qrb3, in0=q3[:, :, 64:96], in1=gqb[:, :, 64:96],
                                                                    op=ALU.mult), r=['qf', 'gq'], w=['srcT_stR'])
                        self.transposes_store(ph, qnp[:, :].rearrange("p (q c) -> p q c", q=8), 8, 128, stQ, 'stQ',
                                              T['QT'][:, :, t * 128:(t + 1) * 128].rearrange("q p n -> p q n"))
                        self.transposes_store(ph, qrb[:, :].rearrange("p (q c) -> p q c", q=8), 8, 64, stR, 'stR',
                                              T['QTr'][:, :, t * 128:(t + 1) * 128].rearrange("q p n -> p q n"))
                    for n, bk in enumerate([0, 1, 2, 6]):
                        ph.op('act', lambda e, n=n, bk=bk: e.copy(out=kvf[:, n * 512:(n + 1) * 512], in_=PS[bk][:, :]),
                              r=[f'ps{bk}'], w=['kvf'])
                    kv3 = kvf[:, :].rearrange("p (h d) -> p h d", d=128)
                    sqk3 = sq[:, 0:1024].rearrange("p (h d) -> p h d", d=64)
                    ph.op('pool', lambda e: e.tensor_tensor(out=sqk3, in0=kv3[:, :, 0:64], in1=kv3[:, :, 0:64], op=ALU.mult),
                          r=['kvf'], w=['sq'])
                    ph.op('dve', lambda e: e.tensor_reduce(out=RKs[:, :], in_=sqk3, axis=AX.X, op=ALU.add), r=['sq'], w=['RKs'])
                    ph.op('act', lambda e: e.activation(out=junk[:, 0:32], in_=kro[:, :], func=AF.Square,
                                                        accum_out=ss[:, 0:1]), r=['kro'], w=['junk', 'ss'])
                    ph.op('dve', lambda e: e.tensor_tensor(out=RKs[:, :], in0=RKs[:, :], in1=ss[:, 0:1].to_broadcast([128, 16]),
                                                           op=ALU.add), r=['RKs', 'ss'], w=['RKs'])
                    self.rstd_ops(ph, RKs[:, :], RKs[:, :], 96, 96 ** -0.5, ['RKs'], ['RKs'])
                    ph.dma('sp', T['RK'][t], RKs[:, :], r=['RKs', 'F1'], w=[], group='stRK')
                    gkb = gk[:, 0:64].unsqueeze(1).broadcast_to([128, 16, 64])
                    kgb3 = kgb[:, :].rearrange("p (h d) -> p h d", d=64)
                    ph.op('dve', lambda e: e.tensor_tensor(out=kgb3, in0=kv3[:, :, 0:64], in1=gkb, op=ALU.mult),
                          r=['kvf', 'gk'], w=['srcT_stK'])
                    ph.op('pool', lambda e: e.tensor_copy(out=vb[:, :].rearrange("p (h d) -> p h d", d=64), in_=kv3[:, :, 64:128]),
                          r=['kvf'], w=['vb'])
                    ph.dma('sp', T['V'][t * 128:(t + 1) * 128, :], vb[:, :], r=['vb', 'F1'], w=[], group='stV')
                    self.transposes_store(ph, kgb[:, :].rearrange("p (q c) -> p q c", q=8), 8, 128, stK, 'stK',
                                          T['KT'][:, :, t * 128:(t + 1) * 128].rearrange("q p n -> p q n"))
                    krg = qtl[:, 0:32]
                    ph.op('dve', lambda e: e.tensor_tensor(out=krg, in0=kro[:, :], in1=gk[:, 64:96], op=ALU.mult),
                          r=['kro', 'gk'], w=['qtl'])
                    krd3 = krd[:, :].rearrange("p (r d) -> p r d", r=2)
                    if lat:
                        self.rope_ops(ph, krg.unsqueeze(1), krd3[:, 0:1, :], rp[:, :, 0:32], 1, 32,
                                      tA[:, 0:32].unsqueeze(1), tB[:, 0:32].unsqueeze(1), ['qtl'], ['krd0', 'srcT_stKr'])
                    else:
                        ph.op('dve', lambda e: e.tensor_copy(out=krd[:, 0:32], in_=krg), r=['qtl'], w=['krd0', 'srcT_stKr'])
                    ph.op('pool', lambda e: e.tensor_copy(out=krd[:, 32:64], in_=krd[:, 0:32]), r=['krd0'], w=['srcT_stKr'])
                    self.transposes_store(ph, krd[:, :].unsqueeze(1), 1, 64, stKr, 'stKr', T['KTr'][:, t * 128:(t + 1) * 128])
                else:
                    if lat:
                        ph.dma('sp', rp[:, :, :], T['rope64'][t], w=['rp'])
                    nb = ncol // 512
                    for n in range(nb):
                        for dc in range(8):
                            ph.op('pe', lambda e, n=n, dc=dc, h3=h3: e.matmul(PS[n][:, :], h3[:, dc, :], Wqkv[:, dc, n * 512:(n + 1) * 512],
                                                                       start=(dc == 0), stop=(dc == 7)),
                                  r=[hT['k'], 'Wqkv'], w=[f'ps{n}'])
                    for n in range(nb):
                        ph.op('act', lambda e, n=n: e.copy(out=qkv[:, n * 512:(n + 1) * 512], in_=PS[n][:, :]),
                              r=[f'ps{n}'], w=['qkv'])
                    Hk = 16 if kind == 1 else 4
                    koff = 1024
                    voff = 2048 if kind == 1 else 1280
                    do_q = lat or need_ctx
                    if do_q:
                        q3 = qkv[:, 0:1024].rearrange("p (h d) -> p h d", d=64)
                        sq3 = sq[:, :].rearrange("p (h d) -> p h d", d=64)
                        ph.op('pool', lambda e: e.tensor_tensor(out=sq3, in0=q3, in1=q3, op=ALU.mult), r=['qkv'], w=['sq'])
                        ph.op('dve', lambda e: e.tensor_reduce(out=st16[:, :], in_=sq3, axis=AX.X, op=ALU.add), r=['sq'], w=['st16'])
                        self.rstd_ops(ph, st16[:, :], st16[:, :], 64, None, ['st16'], ['st16'])
                        qn3 = qn[:, :].rearrange("p (h d) -> p h d", d=64)
                        qb3 = qb[:, :].rearrange("p (h d) -> p h d", d=64)
                        ph.op('dve', lambda e: e.tensor_tensor(out=qn3, in0=q3, in1=st16[:, :].unsqueeze(2).broadcast_to([128, 16, 64]),
                                                               op=ALU.mult), r=['qkv', 'st16'], w=['qn'])
                        gqb = gq[:, :].unsqueeze(1).broadcast_to([128, 16, 64])
                        if lat:
                            ph.op('pool', lambda e: e.tensor_tensor(out=qn3, in0=qn3, in1=gqb, op=ALU.mult), r=['qn', 'gq'], w=['qn'])
                            self.rope_ops(ph, qn3, qb3, rp[:, :, :], 16, 64, tA[:, :].rearrange("p (h d) -> p h d", d=64),
                                          tB[:, :].rearrange("p (h d) -> p h d", d=64), ['qn'], ['srcT_stQ'])
                        else:
                            ph.op('pool', lambda e: e.tensor_tensor(out=qb3, in0=qn3, in1=gqb, op=ALU.mult), r=['qn', 'gq'],
                                  w=['srcT_stQ'])
                        self.transposes_store(ph, qb[:, :].rearrange("p (q c) -> p q c", q=8), 8, 128, stQ, 'stQ',
                                              T['QT'][:, :, t * 128:(t + 1) * 128].rearrange("q p n -> p q n"))
                    k3 = qkv[:, koff:koff + Hk * 64].rearrange("p (h d) -> p h d", d=64)
                    sqk3 = sq[:, 0:Hk * 64].rearrange("p (h d) -> p h d", d=64)
                    ph.op('pool', lambda e: e.tensor_tensor(out=sqk3, in0=k3, in1=k3, op=ALU.mult), r=['qkv'], w=['sq'])
                    ph.op('dve', lambda e: e.tensor_reduce(out=RKs[:, 0:Hk], in_=sqk3, axis=AX.X, op=ALU.add), r=['sq'], w=['RKs'])
                    self.rstd_ops(ph, RKs[:, 0:Hk], RKs[:, 0:Hk], 64, 64 ** -0.5, ['RKs'], ['RKs'])
                    ph.dma('sp', T['RK'][t], RKs[:, :], r=['RKs', 'F1'], w=[], group='stRK')
                    kn3 = kn[:, 0:Hk * 64].rearrange("p (h d) -> p h d", d=64)
                    gkb = gk[:, :].unsqueeze(1).broadcast_to([128, Hk, 64])
                    kkey = ['srcT_stK'] if kind == 1 else ['kb0', 'srcT_stK']
                    if kind == 1:
                        kdst = kb_[:, :].rearrange("p (h d) -> p h d", d=64)
                        kdst2 = None
                    else:
                        k4 = kb_[:, 0:512].rearrange("p (h r d) -> p h r d", r=2, d=64)
                        kdst, kdst2 = k4[:, :, 0, :], k4[:, :, 1, :]
                    if lat:
                        ph.op('pool', lambda e: e.tensor_tensor(out=kn3, in0=k3, in1=gkb, op=ALU.mult), r=['qkv', 'gk'], w=['kn'])
                        self.rope_ops(ph, kn3, kdst, rp[:, :, :], Hk, 64, tA[:, 0:Hk * 64].rearrange("p (h d) -> p h d", d=64),
                                      tB[:, 0:Hk * 64].rearrange("p (h d) -> p h d", d=64), ['kn'], kkey)
                    else:
                        ph.op('pool', lambda e: e.tensor_tensor(out=kdst, in0=k3, in1=gkb, op=ALU.mult), r=['qkv', 'gk'], w=kkey)
                    v3 = qkv[:, voff:voff + (1024 if kind == 1 else 256)]
                    if kind == 1:
                        ph.op('act', lambda e: e.copy(out=vb[:, :], in_=v3), r=['qkv'], w=['vb'])
                        ph.dma('sp', T['V'][t * 128:(t + 1) * 128, :], vb[:, :], r=['vb', 'F1'], w=[], group='stV')
                        self.transposes_store(ph, kb_[:, :].rearrange("p (q c) -> p q c", q=8), 8, 128, stK, 'stK',
                                              T['KT'][:, :, t * 128:(t + 1) * 128].rearrange("q p n -> p q n"))
                    else:
                        ph.op('pool', lambda e: e.tensor_copy(out=kdst2, in_=kdst), r=['kb0'], w=['srcT_stK'])
                        v4 = vb[:, 0:512].rearrange("p (h r d) -> p h r d", r=2, d=64)
                        vv = v3.rearrange("p (h d) -> p h d", d=64)
                        ph.op('act', lambda e: e.copy(out=v4[:, :, 0, :], in_=vv), r=['qkv'], w=['vb0', 'vb'])
                        ph.op('pool', lambda e: e.tensor_copy(out=v4[:, :, 1, :], in_=vv), r=['qkv', 'vb0'], w=['vb'])
                        ph.dma('sp', T['V'][t * 128:(t + 1) * 128, 0:512], vb[:, 0:512], r=['vb', 'F1'], w=[], group='stV')
                        self.transposes_store(ph, kb_[:, 0:512].rearrange("p (q c) -> p q c", q=4), 4, 128, stK, 'stK',
                                              T['KT'][0:4, :, t * 128:(t + 1) * 128].rearrange("q p n -> p q n"))
            fz = sbt("fz", [128, 2], F32)
            ph.op('dve', lambda e: e.memset(fz[:, 0:1], 0.0), w=['F1'])
            self.attention(ph, es, li, kind, need_ctx)
            ph.op('dve', lambda e: e.memset(fz[:, 1:2], 0.0), w=['F2'])
            self.out_proj(ph, es, li, Wo, need_ctx, bc['gx'], bc['gc'], xts, [tA, tB])
            ph.run()

    def phase_peer1(self, li):
        nc, T, PS = self.nc, self.T, self.PS
        last = li == DEPTH - 1
        tiles = list(range(NLAT if last else NT))
        with ExitStack() as es:
            ph = Phase(nc, f"pa{li}")
            sbt = lambda name, shape, dt: es.enter_context(nc.sbuf_tensor(f"pa{li}_{name}", shape, dt))
            bc = {}
            for nm_, who, slot in [('Gx', 0, 4), ('Sx', 0, 3), ('Gc', 1, 4), ('Sc', 1, 3)]:
                tl = sbt(nm_, [128, 1024], F32)
                self.load_bcast(ph, tl, nm_, self.modrow(li, who, slot))
                bc[nm_] = dict(ap=tl[:, :], k=nm_)
            sb = dict(ss=sbt("ss", [128, 4], F32), rs=sbt("rs", [128, 4], F32), junk=sbt("junk", [128, 2048], BF16),
                      tmp=sbt("tmp", [128, 1024], F32), hb=sbt("hb", [128, 1024], BF16))
            xts = [sbt(f"xt{k}", [128, 1024], F32) for k in range(2)]
            hTs = [sbt(f"hT{k}", [128, 1024], BF16) for k in range(2)]
            Wq = sbt("Wq", [128, 8, 2048], BF16)
            keysT = sbt("keysT", [128, 16, 128], BF16)
            ph.dma('pool', Wq[:, :, :], T['peer_w_q'][li], w=['Wq'])
            ph.dma('pool', keysT[:, :, :], T['peer_keys'][li], w=['keysT'])
            qTs = sbt("qTs", [128, 2048], BF16)
            sc = sbt("sc", [128, 2048], F32)
            wk2 = [sbt(f"wk{k}", [128, 256], F32) for k in range(2)]
            sv = sbt("sv", [128, 256], F32)
            si = sbt("si", [128, 256], U32)
            sif = sbt("sif", [128, 256], F32)
            cand = sbt("cand", [128, 2048], F32)
            cv = sbt("cv", [128, 128], F32)
            cp = sbt("cp", [128, 128], U32)
            cpf = sbt("cpf", [128, 128], F32)
            Af = sbt("Af", [128, 128], F32)
            Bf = sbt("Bf", [128, 128], F32)
            eq1 = sbt("eq1", [128, 2048], F32)
            eq2 = sbt("eq2", [128, 2048], F32)
            i1f = sbt("i1f", [128, 128], F32)
            i2f = sbt("i2f", [128, 128], F32)
            ge = sbt("ge", [128, 128], F32)
            gate = sbt("gate", [128, 128], F32)
            sm = sbt("sm", [128, 24], F32)
            tT = [sbt(f"tT{k}", [128, 128], F32) for k in range(3)]
            OI = [sbt(f"OI{k}", [128, 8, 128], BF16) for k in range(2)]
            OJ = [sbt(f"OJ{k}", [128, 8, 128], BF16) for k in range(2)]
            EQ = [sbt(f"EQ{k}", [128, 8, 128], BF16) for k in range(2)]
            Wt = sbt("Wt", [128, 128, 128], BF16)
            iof = self.iota
            def stage_a(n, t):
                lat = t < NLAT
                xb = n % 2
                xt = dict(ap=xts[xb][:, :], k=f'xt{xb}')
                hT = dict(ap=hTs[xb][:, :], k=f'hT{xb}')
                ph.dma('sp', xts[xb][:, :], T['X'][t * 128:(t + 1) * 128, :], w=[f'xt{xb}'])
                self.modulate_tile(ph, sb, t, xt, bc['Gx'] if lat else bc['Gc'], bc['Sx'] if lat else bc['Sc'], hT, 'f')
                h3 = hTs[xb][:, :].rearrange("p (c n) -> p c n", c=8)
                ph.dma('sp', T['H2T'][:, :, t * 128:(t + 1) * 128], h3, r=[hT['k']], w=[], group=f'stH{xb}')
                for hc in range(16):
                    bank, col = hc // 4, (hc % 4) * 128
                    for dc in range(8):
                        ph.op('pe', lambda e, bank=bank, col=col, hc=hc, dc=dc, h3=h3: e.matmul(
                            PS[bank][:, col:col + 128], Wq[:, dc, hc * 128:(hc + 1) * 128], h3[:, dc, :],
                            start=(dc == 0), stop=(dc == 7)), r=['Wq', hT['k']], w=[f'ps{bank}'])
                for bank in range(4):
                    ph.op('act', lambda e, bank=bank: e.copy(out=qTs[:, bank * 512:(bank + 1) * 512], in_=PS[bank][:, :]),
                          r=[f'ps{bank}'], w=['qTs'])
                for hc in range(16):
                    bank, col = 4 + hc // 4, (hc % 4) * 128
                    ph.op('pe', lambda e, bank=bank, col=col, hc=hc: e.matmul(
                        PS[bank][:, col:col + 128], qTs[:, hc * 128:(hc + 1) * 128], keysT[:, hc, :], start=True, stop=True),
                        r=['qTs', 'keysT'], w=[f'ps{bank}'])
                for bank in range(4):
                    ph.op('act', lambda e, bank=bank: e.copy(out=sc[:, bank * 512:(bank + 1) * 512], in_=PS[4 + bank][:, :]),
                          r=[f'ps{4 + bank}'], w=['sc'])


            def stage_b(n, t):
                def top16(src, vals, idxs, wkv, ksrc, kv, ki, kw):
                    ph.op('dve', lambda e: e.max(out=vals[:, 0:8], in_=src), r=[ksrc], w=[kv])
                    ph.op('dve', lambda e: e.max_index(out=idxs[:, 0:8], in_max=vals[:, 0:8], in_values=src),
                          r=[kv, ksrc], w=[ki])
                    ph.op('dve', lambda e: e.match_replace(out=wkv, in_to_replace=vals[:, 0:8], in_values=src,
                                                           imm_value=-1e30), r=[kv, ksrc], w=[kw])
                    ph.op('dve', lambda e: e.max(out=vals[:, 8:16], in_=wkv), r=[kw], w=[kv])
                    ph.op('dve', lambda e: e.max_index(out=idxs[:, 8:16], in_max=vals[:, 8:16], in_values=wkv),
                          r=[kv, kw], w=[ki])

                for hc in range(16):
                    top16(sc[:, hc * 128:(hc + 1) * 128], sv[:, hc * 16:(hc + 1) * 16], si[:, hc * 16:(hc + 1) * 16],
                          wk2[hc % 2][:, 0:128], 'sc', f'sv{hc}', f'si{hc}', f'wk{hc % 2}')
                ph.op('dve', lambda e: e.tensor_copy(out=sif[:, :], in_=si[:, :]), r=[f'si{k}' for k in range(16)], w=['sif'])
                sv4 = sv[:, :].rearrange("p (h c k) -> p h c k", c=2, k=16)
                ph.op('dve', lambda e: e.tensor_tensor(
                    out=cand[:, :].rearrange("p (h a b) -> p h a b", a=16, b=16),
                    in0=sv4[:, :, 0, :].unsqueeze(3).broadcast_to([128, 8, 16, 16]),
                    in1=sv4[:, :, 1, :].unsqueeze(2).broadcast_to([128, 8, 16, 16]), op=ALU.add), r=[f'sv{k}' for k in range(16)], w=['cand'])
                for h in range(8):
                    top16(cand[:, h * 256:(h + 1) * 256], cv[:, h * 16:(h + 1) * 16], cp[:, h * 16:(h + 1) * 16],
                          wk2[h % 2][:, 0:256], 'cand', f'cv{h}', f'cp{h}', f'wk{h % 2}')
                cpu = cpf[:, :].bitcast(U32)
                ph.op('dve', lambda e: e.tensor_scalar(out=cpu, in0=cp[:, :], scalar1=4, scalar2=None,
                                                       op0=ALU.logical_shift_right), r=[f'cp{k}' for k in range(8)], w=['cpf'])
                ph.op('dve', lambda e: e.tensor_copy(out=Af[:, :], in_=cpu), r=['cpf'], w=['Af'])
                ph.op('dve', lambda e: e.tensor_scalar(out=cpu, in0=cp[:, :], scalar1=15, scalar2=None,
                                                       op0=ALU.bitwise_and), r=[f'cp{k}' for k in range(8)] + ['Af'], w=['cpf'])
                ph.op('dve', lambda e: e.tensor_copy(out=Bf[:, :], in_=cpu), r=['cpf'], w=['Bf'])
                sif4 = sif[:, :].rearrange("p (h c k) -> p h c k", c=2, k=16)
                io16 = iof[:, 0:16].unsqueeze(1).unsqueeze(1).broadcast_to([128, 8, 16, 16])
                for (XF, cidx, eq, outf, key) in ((Af, 0, eq1, i1f, 'i1f'), (Bf, 1, eq2, i2f, 'i2f')):
                    eq4 = eq[:, :].rearrange("p (h a b) -> p h a b", a=16, b=16)
                    xb4 = XF[:, :].rearrange("p (h k) -> p h k", k=16).unsqueeze(3).broadcast_to([128, 8, 16, 16])
                    sb4 = sif4[:, :, cidx, :].unsqueeze(2).broadcast_to([128, 8, 16, 16])
                    ph.op('dve', lambda e, eq4=eq4, xb4=xb4: e.tensor_tensor(out=eq4, in0=io16, in1=xb4, op=ALU.is_equal),
                          r=['iota', 'Af', 'Bf'], w=[key + 'e'])
                    ph.op('pool', lambda e, eq4=eq4, sb4=sb4: e.tensor_tensor(out=eq4, in0=eq4, in1=sb4, op=ALU.mult),
                          r=[key + 'e', 'sif'], w=[key + 'e'])
                    ph.op('dve', lambda e, eq=eq, outf=outf: e.tensor_reduce(
                        out=outf[:, :], in_=eq[:, :].rearrange("p (x a) -> p x a", a=16), axis=AX.X, op=ALU.add),
                        r=[key + 'e'], w=[key])
                cv3 = cv[:, :].rearrange("p (h k) -> p h k", k=16)
                ph.op('dve', lambda e: e.tensor_scalar(out=sm[:, 0:8], in0=cv3[:, :, 0], scalar1=-1.0, scalar2=None, op0=ALU.mult),
                      r=[f'cv{k}' for k in range(8)], w=['sm'])
                for h in range(8):
                    ph.op('act', lambda e, h=h: e.activation(out=ge[:, h * 16:(h + 1) * 16], in_=cv[:, h * 16:(h + 1) * 16],
                                                             func=AF.Exp, bias=sm[:, h:h + 1], scale=1.0,
                                                             accum_out=sm[:, 8 + h:9 + h]), r=[f'cv{h}', 'sm'], w=['ge', f'Z{h}'])
                ph.op('dve', lambda e: e.reciprocal(out=sm[:, 16:24], in_=sm[:, 8:16]), r=[f'Z{h}' for h in range(8)], w=['rz'])
                ph.op('dve', lambda e: e.tensor_tensor(out=gate[:, :].rearrange("p (h k) -> p h k", k=16),
                                                       in0=ge[:, :].rearrange("p (h k) -> p h k", k=16),
                                                       in1=sm[:, 16:24].unsqueeze(2).broadcast_to([128, 8, 16]), op=ALU.mult),
                      r=['ge', 'rz'], w=['gate'])
                for k, (src, key) in enumerate(((i1f, 'i1f'), (i2f, 'i2f'), (gate, 'gate'))):
                    ph.op('pe', lambda e, k=k, src=src: e.transpose(PS[4 + k][:, 0:128], src[:, :], self.identf[:, :]),
                          r=[key, 'identf'], w=[f'ps{4 + k}'])
                    ph.op('act', lambda e, k=k: e.copy(out=tT[k][:, :], in_=PS[4 + k][:, 0:128]), r=[f'ps{4 + k}'], w=[f'tT{k}'])

            def stage_c(n, t):
                Wt3 = Wt[:, :, :]
                iob = iof[:, :].unsqueeze(1).broadcast_to([128, 8, 128])
                for g8 in range(16):
                    ob = g8 % 2
                    t0_ = g8 * 8
                    bc0 = tT[0][:, t0_:t0_ + 8].unsqueeze(2).broadcast_to([128, 8, 128])
                    bc1 = tT[1][:, t0_:t0_ + 8].unsqueeze(2).broadcast_to([128, 8, 128])
                    bc2 = tT[2][:, t0_:t0_ + 8].unsqueeze(2).broadcast_to([128, 8, 128])
                    ph.op('dve', lambda e, ob=ob, bc1=bc1: e.tensor_tensor(out=OJ[ob][:, :, :], in0=iob, in1=bc1, op=ALU.is_equal),
                          r=['iota', 'tT1'], w=[f'OJ{ob}'])
                    ph.op('dve', lambda e, ob=ob, bc0=bc0: e.tensor_tensor(out=EQ[ob][:, :, :], in0=iob, in1=bc0, op=ALU.is_equal),
                          r=['iota', 'tT0'], w=[f'EQ{ob}'])
                    ph.op('pool', lambda e, ob=ob, bc2=bc2: e.tensor_tensor(out=OI[ob][:, :, :], in0=EQ[ob][:, :, :], in1=bc2,
                                                                            op=ALU.mult), r=[f'EQ{ob}', 'tT2'], w=[f'OI{ob}'])
                    for u in range(8):
                        tt = t0_ + u
                        bank = (tt // 4) % 4
                        ph.op('pe', lambda e, ob=ob, bank=bank, tt=tt, u=u: e.matmul(
                            PS[bank][:, (tt % 4) * 128:(tt % 4 + 1) * 128], OJ[ob][:, u, :], OI[ob][:, u, :], start=True, stop=True),
                            r=[f'OI{ob}', f'OJ{ob}'], w=[f'ps{bank}'])
                        if tt % 4 == 3:
                            ph.op('act', lambda e, bank=bank, tt=tt: e.copy(
                                out=Wt3[:, :, tt - 3:tt + 1], in_=PS[bank][:, :].rearrange("p (t i) -> p i t", t=4)),
                                r=[f'ps{bank}'], w=['Wt'])
                for k8 in range(8):
                    ph.dma('sp', T['Wd2'][k8 * 16:(k8 + 1) * 16, :, t * 128:(t + 1) * 128].rearrange("i j t -> j i t"),
                           Wt3[:, k8 * 16:(k8 + 1) * 16, :], r=['Wt'], w=[], group='stW')

            stage_a(0, tiles[0])
            for n, t in enumerate(tiles):
                stage_b(n, t)
                if n + 1 < len(tiles):
                    stage_a(n + 1, tiles[n + 1])
                stage_c(n, t)
            ph.run()

    def phase_peer2(self, li):
        nc, T, PS = self.nc, self.T, self.PS
        last = li == DEPTH - 1
        ntile = NLAT if last else NT
        nblk = ntile // 2
        with ExitStack() as es:
            ph = Phase(nc, f"pb{li}")
            sbt = lambda name, shape, dt: es.enter_context(nc.sbuf_tensor(f"pb{li}_{name}", shape, dt))
            Yacc = sbt("Yacc", [128, ntile, 1024], F32)
            UTb = [sbt(f"UT{k}", [128, 8, 512], BF16) for k in range(2)]
            Vb = [sbt(f"Vb{k}", [128, 4, 1024], BF16) for k in range(2)]
            Wg = [sbt(f"Wg{k}", [128, 4, ntile * 128], BF16) for k in range(2)]
            hTb = [sbt(f"hTb{k}", [128, 8, 256], BF16) for k in range(2)]
            gl = [sbt(f"gl{k}", [128, 256], F32) for k in range(4)]
            Am = [sbt(f"Am{k}", [128, 256], BF16) for k in range(4)]
            HB = [0, 1, 6, 7]
            LOOK = 3
            items = []
            it = 0
            for g in range(32):
                for blk in range(nblk):
                    hb = it % 2
                    it += 1
                    for c in range(4):
                        items.append(dict(g=g, b=g % 2, blk=blk, hb=hb, c=c, k=len(items) % 4,
                                          first_g=(blk == 0 and c == 0), first_blk=(c == 0)))

            def emit_H(itm):
                g, b, blk, hb, c, k = (itm[x] for x in ('g', 'b', 'blk', 'hb', 'c', 'k'))
                if itm['first_g']:
                    ph.dma('pool', UTb[b][:, :, :], T['peer_u'][li, g], w=[f'UT{b}'])
                    ph.dma('pool', Vb[b][:, :, :], T['peer_v'][li, g], w=[f'Vb{b}'])
                    ph.dma('sp', Wg[b][:, :, :], T['Wd2'][g * 4:(g + 1) * 4, :, 0:ntile * 128].rearrange("c j t -> j c t"),
                           w=[f'Wg{b}'])
                if itm['first_blk']:
                    ph.dma('sp', hTb[hb][:, :, :], T['H2T'][:, :, blk * 256:(blk + 1) * 256], w=[f'hTb{hb}'])
                for dc in range(8):
                    ph.op('pe', lambda e, dc=dc: e.matmul(PS[HB[k]][:, 0:256], UTb[b][:, dc, c * 128:(c + 1) * 128], hTb[hb][:, dc, :],
                                                          start=(dc == 0), stop=(dc == 7)), r=[f'UT{b}', f'hTb{hb}'], w=[f'ps{HB[k]}'])

            def emit_out(itm):
                g, b, blk, hb, c, k = (itm[x] for x in ('g', 'b', 'blk', 'hb', 'c', 'k'))
                ph.op('act', lambda e: e.activation(out=gl[k][:, :], in_=PS[HB[k]][:, 0:256], func=AF.Gelu),
                      r=[f'ps{HB[k]}'], w=[f'gl{k}'])
                ph.op('dve', lambda e: e.tensor_tensor(out=Am[k][:, :], in0=gl[k][:, :], in1=Wg[b][:, c, blk * 256:(blk + 1) * 256],
                                                       op=ALU.mult), r=[f'gl{k}', f'Wg{b}'], w=[f'Am{k}'])
                for a in range(2):
                    for half in range(2):
                        ph.op('pe', lambda e, a=a, half=half: e.matmul(
                            PS[2 + 2 * a + half][:, :], Am[k][:, a * 128:(a + 1) * 128], Vb[b][:, c, half * 512:(half + 1) * 512],
                            start=(c == 0), stop=(c == 3)), r=[f'Am{k}', f'Vb{b}'], w=[f'ps{2 + 2 * a + half}'])
                if c != 3:
                    return
                for a in range(2):
                    tile = blk * 2 + a
                    for half in range(2):
                        ya = Yacc[:, tile, half * 512:(half + 1) * 512]
                        pk = 2 + 2 * a + half
                        if g == 0:
                            ph.op('act', lambda e, ya=ya, pk=pk: e.copy(out=ya, in_=PS[pk][:, :]), r=[f'ps{pk}'],
                                  w=[f'Y{tile}_{half}'])
                        else:
                            ph.op('dve', lambda e, ya=ya, pk=pk: e.tensor_tensor(out=ya, in0=PS[pk][:, :], in1=ya, op=ALU.add),
                                  r=[f'ps{pk}', f'Y{tile}_{half}'], w=[f'Y{tile}_{half}'])

            for i in range(min(LOOK, len(items))):
                emit_H(items[i])
            for i, itm in enumerate(items):
                if i + LOOK < len(items):
                    emit_H(items[i + LOOK])
                emit_out(itm)
            gx = sbt("gx", [128, 1024], F32)
            gc = sbt("gc", [128, 1024], F32)
            self.load_bcast(ph, gx, 'gx', self.modrow(li, 0, 5))
            self.load_bcast(ph, gc, 'gc', self.modrow(li, 1, 5))
            xo = [sbt(f"xo{k}", [128, 1024], F32) for k in range(2)]
            for t in range(ntile):
                b = t % 2
                g_ = gx if t < NLAT else gc
                gk_ = 'gx' if t < NLAT else 'gc'
                ph.dma('sp', xo[b][:, :], T['X'][t * 128:(t + 1) * 128, :], w=[f'xo{b}'])
                ph.op('dve', lambda e, t=t, g_=g_: e.tensor_tensor(out=Yacc[:, t, :], in0=Yacc[:, t, :], in1=g_[:, :], op=ALU.mult),
                      r=[f'Y{t}_0', f'Y{t}_1', gk_], w=[f'Y{t}_0', f'Y{t}_1'])
                ph.op('pool', lambda e, t=t, b=b: e.tensor_tensor(out=xo[b][:, :], in0=xo[b][:, :], in1=Yacc[:, t, :], op=ALU.add),
                      r=[f'Y{t}_0', f'Y{t}_1', f'xo{b}'], w=[f'xo{b}'])
                dst = T['out'][t * 128:(t + 1) * 128, :] if last else T['X'][t * 128:(t + 1) * 128, :]
                ph.dma('sp', dst, xo[b][:, :], r=[f'xo{b}'], w=[], group=f'stX{b}')
            ph.run()

    def phase_init(self, es):
        nc, T = self.nc, self.T
        sbt = lambda name, shape, dt: es.enter_context(nc.sbuf_tensor(name, shape, dt))
        self.ident = sbt("ident", [128, 128], BF16)
        self.identf = sbt("identf", [128, 128], F32)
        self.ones = sbt("ones", [128, 128], BF16)
        self.iota = sbt("iota", [128, 128], F32)
        self.epsc = sbt("epsc", [128, 1], F32)
        self.PS = [es.enter_context(nc.psum_tensor(f"ps{b}", [128, 512], F32)) for b in range(8)]
        ph = Phase(nc, "init")
        ph.op('dve', lambda e: e.memset(self.epsc[:, :], EPS), w=['epsc'])
        ph.dma('pool', self.ident[:, :], T['identc'], w=['ident'])
        ph.dma('sp', self.identf[:, :], T['identc'], w=['identf'])
        ph.dma('pool', self.ones[:, :], T['onesc'], w=['ones'])
        ph.dma('sp', self.iota[:, :], T['iotac'], w=['iota'])
        for t in range(NT):
            ph.dma('sp', T['X'][t * 128:(t + 1) * 128, :], T['xin'][t * 128:(t + 1) * 128, :], w=[f'X{t}'], group=f'xc{t % 4}')
        ph.run()

    def build(self):
        nc = self.nc
        L = self.layers
        kinds = set(l % 3 for l in L)
        self.din('xin', [TOK, D]); self.din('cvec', [128, 16])
        self.din('ada_w', [DEPTH, 12, 128, 8, 512]); self.din('ada_b', [DEPTH, 6144])
        self.din('norm_mix', [DEPTH, D]); self.din('norm_ffn', [DEPTH, D])
        self.din('identc', [128, 128]); self.din('onesc', [128, 128]); self.din('iotac', [128, 128])
        if 0 in kinds:
            self.din('mla_w_down', [2, 128, 8, 1056]); self.din('mla_w_uq', [2, 128, 6, 1536])
            self.din('mla_w_ukv', [2, 128, 2, 2048]); self.din('mla_w_o', [2, 128, 8, 1024])
            self.din('mla_g_cq', [2, 768]); self.din('mla_g_ckv', [2, 256]); self.din('mla_g_q', [2, 96]); self.din('mla_g_k', [2, 96])
            self.din('rope32', [NLAT, 128, 2, 32])
        if 1 in kinds:
            self.din('diff_w_qkv', [128, 8, 3072]); self.din('diff_w_o', [128, 8, 1024])
            self.din('diff_g_q', [64]); self.din('diff_g_k', [64]); self.din('diff_lambda', [256]); self.din('diff_g_sub', [128])
        if 2 in kinds:
            self.din('swa_w_qkv', [128, 8, 1536]); self.din('swa_w_o', [128, 8, 1024])
            self.din('swa_g_q', [64]); self.din('swa_g_k', [64]); self.din('swa_sink', [16]); self.din('swamask', [6, 128, 512])
        if 1 in kinds or 2 in kinds:
            self.din('rope64', [NLAT, 128, 2, 64])
        if self.peer:
            self.din('peer_w_q', [DEPTH, 128, 8, 2048]); self.din('peer_keys', [DEPTH, 128, 16, 128])
            self.din('peer_u', [DEPTH, 32, 128, 8, 512]); self.din('peer_v', [DEPTH, 32, 128, 4, 1024])
        full_out = getattr(self, 'full_out', False)
        self.T['out'] = nc.dram_tensor('out', [TOK if full_out else NLAT * 128, D], F32, kind="ExternalOutput").ap()
        self.dscr('X', [TOK, D], F32); self.dscr('modrows', [DEPTH, 2, 6144], F32)
        self.dscr('QT', [8, 128, TOK], BF16); self.dscr('QTr', [8, 64, TOK], BF16); self.dscr('KT', [8, 128, TOK], BF16)
        self.dscr('KTr', [64, TOK], BF16); self.dscr('V', [TOK, D], BF16); self.dscr('RK', [NT, 128, 16], F32)
        self.dscr('OT', [8, 128, TOK], BF16); self.dscr('H2T', [128, 8, TOK], BF16)
        if self.peer:
            self.dscr('Wd2', [128, 128, TOK], BF16)
        with ExitStack() as es:
            self.phase_init(es)
            for li in L:
                self.phase_mod(li)
                self.phase_mixer(li)
                if self.peer:
                    self.phase_peer1(li)
                    self.phase_peer2(li)
            if self.dbg or not (self.peer and (DEPTH - 1) in L):
                ph = Phase(nc, "dump")
                for t in range(NT if full_out else NLAT):
                    ph.dma('sp', self.T['out'][t * 128:(t + 1) * 128, :], self.T['X'][t * 128:(t + 1) * 128, :], w=[f'o{t}'],
                           group=f'dm{t % 4}')
                ph.run()
        return nc


def prep_shared(inp, layers, peer):
    f = lambda a: np.ascontiguousarray(a, dtype=np.float32)
    kinds = set(l % 3 for l in layers)
    sh = {}
    sh['ada_w'] = f(inp['ada_w'].reshape(DEPTH, 8, 128, 12, 512).transpose(0, 3, 2, 1, 4))
    sh['ada_b'] = f(inp['ada_b']); sh['norm_mix'] = f(inp['norm_mix']); sh['norm_ffn'] = f(inp['norm_ffn'])
    sh['identc'] = np.eye(128, dtype=np.float32); sh['onesc'] = np.ones((128, 128), np.float32)
    sh['iotac'] = f(np.tile(np.arange(128, dtype=np.float32)[None, :], (128, 1)))
    chunk = lambda w, k: f(w.reshape(w.shape[0], k, 128, w.shape[2]).transpose(0, 2, 1, 3))
    if 0 in kinds:
        sh['mla_w_down'] = chunk(inp['mla_w_down'], 8); sh['mla_w_uq'] = chunk(inp['mla_w_uq'], 6)
        sh['mla_w_ukv'] = chunk(inp['mla_w_ukv'], 2); sh['mla_w_o'] = chunk(inp['mla_w_o'], 8)
        for k in ('mla_g_cq', 'mla_g_ckv', 'mla_g_q', 'mla_g_k'):
            sh[k] = f(inp[k])
        sh['rope32'] = rope_tables(32)
    if 1 in kinds:
        sh['diff_w_qkv'] = chunk(inp['diff_w_qkv'], 8)[0]; sh['diff_w_o'] = chunk(inp['diff_w_o'], 8)[0]
        sh['diff_g_q'] = f(inp['diff_g_q'][0]); sh['diff_g_k'] = f(inp['diff_g_k'][0])
        sh['diff_lambda'] = f(inp['diff_lambda'][0].reshape(256)); sh['diff_g_sub'] = f(inp['diff_g_sub'][0])
    if 2 in kinds:
        sh['swa_w_qkv'] = chunk(inp['swa_w_qkv'], 8)[0]; sh['swa_w_o'] = chunk(inp['swa_w_o'], 8)[0]
        sh['swa_g_q'] = f(inp['swa_g_q'][0]); sh['swa_g_k'] = f(inp['swa_g_k'][0])
        sh['swa_sink'] = f(inp['swa_sink'][0].reshape(16)); sh['swamask'] = swa_masks()
    if 1 in kinds or 2 in kinds:
        sh['rope64'] = rope_tables(64)
    if peer:
        sh['peer_w_q'] = chunk(inp['peer_w_q'], 8)
        sh['peer_keys'] = f(inp['peer_keys'].transpose(0, 4, 1, 2, 3).reshape(DEPTH, 128, 16, 128))
        sh['peer_u'] = f(inp['peer_u'].reshape(DEPTH, 32, 512, 8, 128).transpose(0, 1, 4, 3, 2))
        sh['peer_v'] = f(inp['peer_v'].reshape(DEPTH, 32, 4, 128, 1024).transpose(0, 1, 3, 2, 4))
    return sh


def prep_core(inp, b):
    f = lambda a: np.ascontiguousarray(a, dtype=np.float32)
    d = {}
    d['xin'] = f(np.concatenate([inp['x'][b], inp['ctx'][b]], axis=0))
    cv = np.stack([inp['c'][b].reshape(8, 128), inp['c_ctx'].reshape(8, 128)], axis=-1)
    d['cvec'] = f(cv.transpose(1, 0, 2).reshape(128, 16))
    return d


def run(inp, layers=(0, 1, 2, 3), peer=True, cores=NCORES, dbg=False, trace=False):
    bld = Builder(list(layers), dbg)
    bld.peer = peer
    nc = bld.build()
    sh = prep_shared(inp, layers, peer)
    in_maps = []
    for b in range(cores):
        m = dict(sh)
        m.update(prep_core(inp, b))
        in_maps.append(m)
    res = run_bass_kernel_spmd(nc, in_maps, core_ids=list(range(cores)), **({'trace': True} if trace else {}))
    return np.stack([r['out'] for r in res.results], axis=0), res


def run_unfused(inp, cores=NCORES):
    sh = prep_shared(inp, (0, 1, 2, 3), True)
    xin = [prep_core(inp, b) for b in range(cores)]
    out = None
    for li in range(DEPTH):
        bld = Builder([li], False)
        bld.peer = True
        bld.full_out = li < DEPTH - 1
        nc = bld.build()
        names = set(bld.T.keys())
        in_maps = []
        for b in range(cores):
            m = {k: v for k, v in sh.items() if k in names}
            m.update(xin[b])
            in_maps.append(m)
        res = run_bass_kernel_spmd(nc, in_maps, core_ids=list(range(cores)))
        if li < DEPTH - 1:
            for b in range(cores):
                xin[b]['xin'] = np.ascontiguousarray(res.results[b]['out'], dtype=np.float32)
        else:
            out = np.stack([r['out'] for r in res.results], axis=0)
    return out


FUSED = True


def kernel(**inputs):
    inp = {k: np.asarray(v) for k, v in inputs.items()}
    if FUSED:
        out, _ = run(inp)
    else:
        out = run_unfused(inp)
    return out.astype(np.float32)
```
